# Optimizing a Trainium2 kernel written in Bass

```python
import math
import jax, jax.numpy as jnp
from jax import lax
import numpy as np

D_MODEL = 1024
BATCH = 8
SEQ = 4096
DEPTH = 2

D_MIX = D_MODEL
GLA_HEADS = 4
GLA_DV = 64
GLA_DK = GLA_DV // 2
GLA_GATE_RANK = 16
GLA_TAU = 16.0
GLA_CHUNK = 64
CONV_CH = D_MIX // 4
CONV_WIDTH = 31
SWA_Q_HEADS = 8
SWA_KV_HEADS = 2
SWA_HEAD_DIM = 64
SWA_WINDOW = 128
SWA_BLOCK = 128
D_FF = 2816
RMS_EPS = 1e-6
LN_EPS = 1e-5

kernel_name = "hybrid_gla_conformer_swa_macaron"


def _in_proj_widths():
    return [
        GLA_HEADS * GLA_DK,
        GLA_HEADS * GLA_DK,
        GLA_HEADS * GLA_DV,
        GLA_HEADS * GLA_DV,
        GLA_GATE_RANK,
        CONV_CH,
        CONV_CH,
        SWA_Q_HEADS * SWA_HEAD_DIM,
        SWA_KV_HEADS * SWA_HEAD_DIM,
        SWA_KV_HEADS * SWA_HEAD_DIM,
    ]


def rmsnorm(x, g):
    x32 = x.astype(jnp.float32)
    y = x32 * lax.rsqrt(jnp.mean(x32 * x32, axis=-1, keepdims=True) + RMS_EPS)
    return (y * g.astype(jnp.float32)).astype(x.dtype)


def swiglu_ffn(h, w_gate, w_up, w_down):
    return (jax.nn.silu(h @ w_gate) * (h @ w_up)) @ w_down


def gla_group(q, k, v, glog, r, norm_g):
    B, S, H, DK = q.shape
    DV = v.shape[-1]
    C = GLA_CHUNK
    N = S // C
    f32 = jnp.float32

    def chunks(t):
        return t.astype(f32).reshape(B, N, C, H, t.shape[-1]).transpose(1, 0, 3, 2, 4)

    qc = chunks(q) * (DK ** -0.5)
    kc = chunks(k)
    vc = chunks(v)
    bc = jnp.cumsum(chunks(glog), axis=3)
    causal = jnp.tril(jnp.ones((C, C), dtype=bool))[:, :, None]

    def step(state, inp):
        qn, kn, vn, bn = inp
        diff = bn[:, :, :, None, :] - bn[:, :, None, :, :]
        decay = jnp.where(causal, jnp.exp(jnp.where(causal, diff, 0.0)), 0.0)
        attn = jnp.einsum('bhid,bhjd,bhijd->bhij', qn, kn, decay)
        o = jnp.einsum('bhij,bhjv->bhiv', attn, vn) \
            + jnp.einsum('bhid,bhdv->bhiv', qn * jnp.exp(bn), state)
        b_last = bn[:, :, -1:, :]
        state = jnp.exp(b_last[:, :, 0, :])[..., None] * state \
            + jnp.einsum('bhjd,bhjv->bhdv', kn * jnp.exp(b_last - bn), vn)
        return state, o

    s0 = jnp.zeros((B, H, DK, DV), f32)
    _, o = lax.scan(step, s0, (qc, kc, vc, bc))
    o = o.transpose(1, 0, 3, 2, 4).reshape(B, S, H, DV)
    o = o * lax.rsqrt(jnp.mean(o * o, axis=-1, keepdims=True) + RMS_EPS) * norm_g.astype(f32)
    o = o.reshape(B, S, H * DV) * jax.nn.silu(r.astype(f32))
    return o.astype(q.dtype)


def conv_group(a, g, conv_w, conv_b, ln_g, ln_b):
    u = a * jax.nn.sigmoid(g)
    y = lax.conv_general_dilated(
        u, conv_w[:, None, :].astype(u.dtype), window_strides=(1,),
        padding=[(CONV_WIDTH - 1, 0)], dimension_numbers=('NWC', 'WIO', 'NWC'),
        feature_group_count=CONV_CH) + conv_b
    y32 = y.astype(jnp.float32)
    mu = jnp.mean(y32, axis=-1, keepdims=True)
    var = jnp.mean(jnp.square(y32 - mu), axis=-1, keepdims=True)
    y32 = (y32 - mu) * lax.rsqrt(var + LN_EPS) * ln_g.astype(jnp.float32) + ln_b.astype(jnp.float32)
    return jax.nn.silu(y32).astype(a.dtype)


def swa_group(q, k, v, sinks):
    B, S, _ = q.shape
    BLK, HKV, DH = SWA_BLOCK, SWA_KV_HEADS, SWA_HEAD_DIM
    G = SWA_Q_HEADS // HKV
    nb = S // BLK
    qb = q.reshape(B, nb, BLK, HKV, G, DH)
    kb = k.reshape(B, nb, BLK, HKV, DH)
    vb = v.reshape(B, nb, BLK, HKV, DH)

    def with_prev(t):
        prev = jnp.pad(t, ((0, 0), (1, 0), (0, 0), (0, 0), (0, 0)))[:, :-1]
        return jnp.concatenate([prev, t], axis=2)

    kk, vv = with_prev(kb), with_prev(vb)
    s = jnp.einsum('bnqhgd,bnkhd->bnhgqk', qb, kk).astype(jnp.float32) * (DH ** -0.5)
    qi = jnp.arange(BLK)[:, None]
    kj = jnp.arange(2 * BLK)[None, :]
    dist = qi - kj + BLK
    blk = jnp.arange(nb)[:, None, None]
    valid = (dist >= 0) & (dist < SWA_WINDOW) & ((blk > 0) | (kj >= BLK))
    slopes = jnp.exp2(-8.0 * (jnp.arange(SWA_Q_HEADS, dtype=jnp.float32) + 1.0) / SWA_Q_HEADS)
    slopes = slopes.reshape(HKV, G)
    s = s - slopes[:, :, None, None] * dist.astype(jnp.float32)
    s = jnp.where(valid[None, :, None, None], s, -jnp.inf)
    sink = jnp.broadcast_to(sinks.astype(jnp.float32).reshape(HKV, G)[None, None, :, :, None, None],
                            s.shape[:-1] + (1,))
    p = jax.nn.softmax(jnp.concatenate([s, sink], axis=-1), axis=-1)[..., :-1]
    o = jnp.einsum('bnhgqk,bnkhd->bnqhgd', p.astype(v.dtype), vv)
    return o.reshape(B, S, SWA_Q_HEADS * DH)


def hybrid_mixer(h, w_in, gla_w_gate2, gla_b_gate, gla_norm_g, conv_w, conv_b,
                 conv_ln_g, conv_ln_b, swa_sinks, w_out):
    B, S, _ = h.shape
    p = h @ w_in
    offsets = np.cumsum(_in_proj_widths())[:-1].tolist()
    gq, gk, gv, gr, glr, ca, cg, sq, sk, sv = jnp.split(p, offsets, axis=-1)
    glog = jax.nn.log_sigmoid((glr @ gla_w_gate2 + gla_b_gate).astype(jnp.float32)) / GLA_TAU
    o_gla = gla_group(gq.reshape(B, S, GLA_HEADS, GLA_DK), gk.reshape(B, S, GLA_HEADS, GLA_DK),
                      gv.reshape(B, S, GLA_HEADS, GLA_DV), glog.reshape(B, S, GLA_HEADS, GLA_DK),
                      gr, gla_norm_g)
    o_conv = conv_group(ca, cg, conv_w, conv_b, conv_ln_g, conv_ln_b)
    o_swa = swa_group(sq, sk, sv, swa_sinks)
    return jnp.concatenate([o_gla, o_conv, o_swa], axis=-1) @ w_out


def setup_inputs(seed: int = 0) -> dict:
    key = jax.random.key(seed)
    ks = jax.random.split(key, 16)
    f32 = jnp.float32
    d_in = sum(_in_proj_widths())
    nrm = lambda k, shape, scale: jax.random.normal(k, shape, f32) * scale
    return {
        "x": jax.random.normal(ks[0], (BATCH, SEQ, D_MODEL), f32),
        "norm_g": 1.0 + nrm(ks[1], (DEPTH, 6, D_MODEL), 0.05),
        "ffn_w_gate": nrm(ks[2], (DEPTH, 2, D_MODEL, D_FF), D_MODEL ** -0.5),
        "ffn_w_up": nrm(ks[3], (DEPTH, 2, D_MODEL, D_FF), D_MODEL ** -0.5),
        "ffn_w_down": nrm(ks[4], (DEPTH, 2, D_FF, D_MODEL), D_FF ** -0.5),
        "w_in": nrm(ks[5], (DEPTH, D_MODEL, d_in), D_MODEL ** -0.5),
        "gla_w_gate2": nrm(ks[6], (DEPTH, GLA_GATE_RANK, GLA_HEADS * GLA_DK), GLA_GATE_RANK ** -0.5),
        "gla_b_gate": nrm(ks[7], (DEPTH, GLA_HEADS * GLA_DK), 0.1),
        "gla_norm_g": 1.0 + nrm(ks[8], (DEPTH, GLA_HEADS, GLA_DV), 0.05),
        "conv_w": nrm(ks[9], (DEPTH, CONV_WIDTH, CONV_CH), CONV_WIDTH ** -0.5),
        "conv_b": nrm(ks[10], (DEPTH, CONV_CH), 0.02),
        "conv_ln_g": 1.0 + nrm(ks[11], (DEPTH, CONV_CH), 0.05),
        "conv_ln_b": nrm(ks[12], (DEPTH, CONV_CH), 0.02),
        "swa_sinks": nrm(ks[13], (DEPTH, SWA_Q_HEADS), 0.5),
        "w_out": nrm(ks[14], (DEPTH, D_MIX, D_MODEL), D_MIX ** -0.5),
    }


def reference(x, norm_g, ffn_w_gate, ffn_w_up, ffn_w_down, w_in, gla_w_gate2, gla_b_gate,
              gla_norm_g, conv_w, conv_b, conv_ln_g, conv_ln_b, swa_sinks, w_out):
    for l in range(DEPTH):
        g = norm_g[l]
        h = swiglu_ffn(rmsnorm(x, g[0]), ffn_w_gate[l, 0], ffn_w_up[l, 0], ffn_w_down[l, 0])
        x = x + 0.5 * rmsnorm(h, g[1])
        h = hybrid_mixer(rmsnorm(x, g[2]), w_in[l], gla_w_gate2[l], gla_b_gate[l], gla_norm_g[l],
                         conv_w[l], conv_b[l], conv_ln_g[l], conv_ln_b[l], swa_sinks[l], w_out[l])
        x = x + rmsnorm(h, g[3])
        h = swiglu_ffn(rmsnorm(x, g[4]), ffn_w_gate[l, 1], ffn_w_up[l, 1], ffn_w_down[l, 1])
        x = x + 0.5 * rmsnorm(h, g[5])
    return x
```

```python
import contextlib
import numpy as np
import concourse.bass as bass
import concourse.mybir as mybir
from concourse.bass_utils import run_bass_kernel_spmd

F32 = mybir.dt.float32
BF16 = mybir.dt.bfloat16
AF = mybir.ActivationFunctionType
ALU = mybir.AluOpType
AX = mybir.AxisListType

_DSZ = {F32: 4, BF16: 2}

D = 1024
KD = 8
FF = 2816
KF = 22
NEG = -30000.0


def _region(ap):
    t = ap.tensor
    kind = type(t).__name__
    dims = ap.ap
    off = int(ap.offset)
    esz = _DSZ[ap.dtype]
    if kind.startswith("DRam"):
        ext = sum((c - 1) * abs(s) for s, c in dims) + 1
        return (t.name, 0, 1, off * esz, (off + ext) * esz)
    pstep, npart = dims[0]
    if pstep <= 0:
        p_lo, p_hi, fo = 0, 128, off
    else:
        p_lo = off // pstep
        p_hi = p_lo + npart
        fo = off % pstep
    if kind.startswith("PSum"):
        return (t.name, 0, 128, 0, 1 << 30)
    ext = sum((c - 1) * abs(s) for s, c in dims[1:]) + 1
    return (t.name, p_lo, p_hi, fo * esz, (fo + ext) * esz)


def _overlap(a, b):
    return a[1] < b[2] and b[1] < a[2] and a[3] < b[4] and b[3] < a[4]


def _covers(a, b):
    return a[1] <= b[1] and a[2] >= b[2] and a[3] <= b[3] and a[4] >= b[4]


class Prog:
    ENGS = ("pe", "act", "dve", "pool", "sp")

    def __init__(self, nc):
        self.nc = nc
        self.ops = {e: [] for e in self.ENGS}
        self.wrec = {}
        self.rrec = {}
        self.dma_cnt = {}
        self.same_eng_sync = {"act", "dve", "pool"}

    def op(self, eng, fn, reads=(), writes=(), chan=None, group=False):
        deps_e = {}
        deps_d = {}

        def add(tok):
            if tok[0] == "e":
                _, e, i = tok
                if e == eng and e not in self.same_eng_sync:
                    return
                if deps_e.get(e, -1) < i:
                    deps_e[e] = i
            else:
                _, c, n = tok
                if deps_d.get(c, 0) < n:
                    deps_d[c] = n

        rregs = [_region(a) for a in reads]
        wregs = [_region(a) for a in writes]
        for r in rregs:
            for (reg, tok) in self.wrec.get(r[0], ()):
                if _overlap(reg, r):
                    add(tok)
            if r[0].startswith("bank"):
                for (reg, tok) in self.rrec.get(r[0], ()):
                    if tok[0] == "e" and tok[1] != eng:
                        add(tok)
        for w in wregs:
            for (reg, tok) in self.wrec.get(w[0], ()):
                if _overlap(reg, w):
                    add(tok)
            for (reg, tok) in self.rrec.get(w[0], ()):
                if _overlap(reg, w):
                    add(tok)
        idx = len(self.ops[eng])
        if chan is None:
            tok = ("e", eng, idx)
        else:
            n = self.dma_cnt.get(chan, 0) + 1
            self.dma_cnt[chan] = n
            tok = ("d", chan, (1 << 30) if group else n)
        for r in rregs:
            lst = self.rrec.setdefault(r[0], [])
            if tok[0] == "e":
                lst[:] = [(g, t) for (g, t) in lst
                          if not (t[0] == "e" and t[1] == eng and _covers(r, g))]
            lst.append((r, tok))
        for w in wregs:
            wl = self.wrec.setdefault(w[0], [])
            wl[:] = [(g, t) for (g, t) in wl if not _covers(w, g)]
            wl.append((w, tok))
            rl = self.rrec.get(w[0])
            if rl:
                rl[:] = [(g, t) for (g, t) in rl if not _covers(w, g)]
        self.ops[eng].append(dict(fn=fn, de=deps_e, dd=deps_d, chan=chan, sig=False))
        return tok

    def emit(self, final_wait_chans=()):
        nc = self.nc
        ops = self.ops
        for e in self.ENGS:
            for o in ops[e]:
                for (f, i) in o["de"].items():
                    ops[f][i]["sig"] = True
        cum = {}
        for e in self.ENGS:
            c = 0
            arr = []
            for o in ops[e]:
                if o["sig"]:
                    c += 1
                arr.append(c)
            cum[e] = arr
        chans = sorted(self.dma_cnt.keys())
        with contextlib.ExitStack() as st:
            esem = {e: st.enter_context(nc.semaphore("s_" + e)) for e in self.ENGS}
            dsem = {c: st.enter_context(nc.semaphore("d_" + str(c))) for c in chans}
            block = st.enter_context(nc.Block())

            def run(e, h):
                waited_e = {}
                waited_d = {}
                for o in ops[e]:
                    for (f, i) in o["de"].items():
                        tgt = cum[f][i]
                        if waited_e.get(f, 0) < tgt:
                            h.wait_ge(esem[f], tgt)
                            waited_e[f] = tgt
                    for (c, n) in o["dd"].items():
                        n = min(n, self.dma_cnt[c])
                        if waited_d.get(c, 0) < n:
                            h.wait_ge(dsem[c], 16 * n)
                            waited_d[c] = n
                    ins = o["fn"](h)
                    if o["chan"] is not None:
                        ins.then_inc(dsem[o["chan"]], 16)
                    if o["sig"]:
                        ins.then_inc(esem[e], 1)
                if e == "sp":
                    for c in final_wait_chans:
                        h.wait_ge(dsem[c], 16 * self.dma_cnt[c])

            @block.tensor
            def _(h):
                run("pe", h)

            @block.scalar
            def _(h):
                run("act", h)

            @block.vector
            def _(h):
                run("dve", h)

            @block.gpsimd
            def _(h):
                run("pool", h)

            @block.sync
            def _(h):
                run("sp", h)


C_ID, C_ONE, C_TRIN, C_TRIC, C_MASK, C_BIAS = 0, 128, 256, 384, 512, 640
NCONST = 640 + 8 * 256


_HPERM = [0, 2, 1, 3, 4, 6, 5, 7]


def _const_table():
    c = np.zeros((128, NCONST), np.float32)
    j = np.arange(128)[:, None]
    i = np.arange(128)[None, :]
    c[:, C_ID:C_ID + 128] = (j == i)
    c[:, C_ONE:C_ONE + 128] = 1.0
    c[:, C_TRIN:C_TRIN + 128] = np.where(j <= i, -1.0 / 16.0, 0.0)
    c[:, C_TRIC:C_TRIC + 128] = np.where(j > i, -1.0 / 16.0, 0.0)
    c[:, C_MASK:C_MASK + 128] = (j <= i)
    q = np.arange(128)[:, None]
    kj = np.arange(256)[None, :]
    dist = q - kj + 128
    valid = (dist >= 0) & (dist < 128)
    for jpos, h in enumerate(_HPERM):
        slope = 2.0 ** (-(h + 1))
        c[:, C_BIAS + jpos * 256:C_BIAS + (jpos + 1) * 256] = np.where(valid, -slope * dist, NEG)
    return c


_O_GQ, _O_GK, _O_GV, _O_GR, _O_GLR, _O_CA, _O_CG, _O_SQ, _O_SK, _O_SV = (
    0, 128, 256, 512, 768, 784, 1040, 1296, 1808, 1936)

PP_G, PP_CW, PP_CB, PP_LG, PP_LB, NPP = 0, 48, 110, 112, 114, 116


def _fm(v, nch):
    return np.ascontiguousarray(v.reshape(nch, 128).T)


def _prep_weights(inp, L):
    f32 = np.float32
    wgu = np.empty((L, 2, KF, 128, 2048), f32)
    wd = np.empty((L, 2, KD, 128, KF * 128), f32)
    win = np.zeros((L, 10, 128, 2048), f32)
    wo = np.zeros((L, KD, 128, 12 * 128), f32)
    pp = np.zeros((L, 128, NPP), f32)
    pb = np.zeros((L, 264), f32)
    w2 = np.ascontiguousarray(inp["gla_w_gate2"][:L]).astype(f32)
    bg = np.ascontiguousarray(inp["gla_b_gate"][:L]).astype(f32).reshape(L, 1, 128)
    for l in range(L):
        for j in range(2):
            g = inp["ffn_w_gate"][l, j].reshape(KD, 128, KF, 128)
            u = inp["ffn_w_up"][l, j].reshape(KD, 128, KF, 128)
            wgu[l, j] = np.stack([g, u], 0).transpose(3, 2, 0, 1, 4).reshape(KF, 128, 2048)
            dn = inp["ffn_w_down"][l, j].reshape(KF, 128, KD, 128)
            wd[l, j] = dn.transpose(2, 1, 0, 3).reshape(KD, 128, KF * 128)
        w = inp["w_in"][l]
        cols = [w[:, _O_GQ:_O_GQ + 128], w[:, _O_GK:_O_GK + 128]]
        cols += [w[:, _O_CA + 128 * i:_O_CA + 128 * (i + 1)] for i in range(2)]
        cols += [w[:, _O_CG + 128 * i:_O_CG + 128 * (i + 1)] for i in range(2)]
        cols += [w[:, _O_SQ + 128 * i:_O_SQ + 128 * (i + 1)] for i in range(4)]
        for kv in range(2):
            k = w[:, _O_SK + 64 * kv:_O_SK + 64 * (kv + 1)]
            cols.append(np.concatenate([k, k], 1))
        glr = np.zeros((D, 128), f32)
        glr[:, :16] = w[:, _O_GLR:_O_GLR + 16]
        cols.append(glr)
        cols.append(np.zeros((D, 128), f32))
        for un in range(7):
            a = cols[2 * un].reshape(KD, 128, 128)
            b = cols[2 * un + 1].reshape(KD, 128, 128)
            win[l, un] = np.stack([a, b], 0).transpose(2, 0, 1, 3).reshape(128, 2048)
        tm = [np.concatenate([w[:, _O_GK:_O_GK + 128], w[:, _O_SV:_O_SV + 128]], 1),
              w[:, _O_GV:_O_GV + 256], w[:, _O_GR:_O_GR + 256]]
        for un in range(3):
            win[l, 7 + un] = tm[un].reshape(KD, 128, 256).transpose(1, 0, 2).reshape(128, 2048)
        o = inp["w_out"][l]
        for dc in range(KD):
            blk = np.zeros((128, 12, 128), f32)
            oc = o[:, dc * 128:(dc + 1) * 128]
            blk[:, 0:4, :] = oc[0:512].reshape(4, 128, 128).transpose(1, 0, 2)
            blk[0:64, 4:12, :] = oc[512:1024].reshape(8, 64, 128).transpose(1, 0, 2)
            wo[l, dc] = blk.reshape(128, 1536)
        pp[l, :, PP_G:PP_G + 48] = inp["norm_g"][l].reshape(6, KD, 128).transpose(2, 0, 1).reshape(128, 48)
        pp[l, :, PP_CW:PP_CW + 62] = inp["conv_w"][l].reshape(31, 2, 128).transpose(2, 1, 0).reshape(128, 62)
        pp[l, :, PP_CB:PP_CB + 2] = _fm(inp["conv_b"][l], 2)
        pp[l, :, PP_LG:PP_LG + 2] = _fm(inp["conv_ln_g"][l], 2)
        pp[l, :, PP_LB:PP_LB + 2] = _fm(inp["conv_ln_b"][l], 2)
        pb[l, 0:256] = inp["gla_norm_g"][l].reshape(256)
        pb[l, 256:264] = inp["swa_sinks"][l][_HPERM]
    return dict(wgu=wgu, wd=wd, win=win, wo=wo, pp=pp, pb=pb.reshape(L, 1, 264), w2=w2, bg=bg)


import os
POOL = os.environ.get("K_POOL", "pool")


def build(S, L, T=512, dbg=None):
    assert S % T == 0 and T % 512 == 0
    NT = S // T
    NTC = T // 512
    nc = bass.Bass("TRN2", target_bir_lowering=False)
    P = Prog(nc)

    def dram(name, shape, dt=F32, kind="ExternalInput"):
        return nc.dram_tensor(name, list(shape), dt, kind=kind).ap()

    xT = dram("xT", [D, S])
    outT = dram("outT", [D, S], kind="ExternalOutput")
    cst_d = dram("cst", [128, NCONST])
    pp_d = dram("pp", [L, 128, NPP])
    pb_d = dram("pb", [L, 1, 264])
    w2_d = dram("w2", [L, 16, 128])
    bg_d = dram("bg", [L, 1, 128])
    wgu_d = dram("wgu", [L, 2, KF, 128, 2048])
    wd_d = dram("wd", [L, 2, KD, 128, KF * 128])
    win_d = dram("win", [L, 10, 128, 2048])
    wo_d = dram("wo", [L, KD, 128, 1536])
    wgu_b = dram("wgu_b", [L, 2, KF, 128, 2048], BF16, kind="Internal")
    wd_b = dram("wd_b", [L, 2, KD, 128, KF * 128], BF16, kind="Internal")
    win_b = dram("win_b", [L, 10, 128, 2048], BF16, kind="Internal")
    wo_b = dram("wo_b", [L, KD, 128, 1536], BF16, kind="Internal")

    def sb(name, shape, dt=F32):
        return nc.alloc_sbuf_tensor(name, list(shape), dt).ap()

    X = sb("X", [128, KD, T])
    xn = sb("xn", [128, KD, T], BF16)
    U = sb("U", [128, 15360])
    Ub = U.bitcast(BF16)
    hT = Ub[:, 0:KF * T].rearrange("p (f t) -> p f t", f=KF)
    hb = U[:, 11264:11264 + KD * T].rearrange("p (c t) -> p c t", c=KD)
    sqpre = Ub[:, 0:KD * T].rearrange("p (c t) -> p c t", c=KD)

    NW = 4
    wslots = [sb(f"wsl{i}", [128, 3072], BF16) for i in range(NW)]
    NST = 2
    stages = [sb(f"stg{i}", [128, 2048]) for i in range(NST)]
    cst = sb("cstf", [128, NCONST])
    cbf = sb("cstb", [128, 640], BF16)
    ident_b = cbf[:, C_ID:C_ID + 128]
    ones_b = cbf[:, C_ONE:C_ONE + 128]
    mask_b = cbf[:, C_MASK:C_MASK + 128]
    ones_f = cst[:, C_ONE:C_ONE + 128]
    trin_f = cst[:, C_TRIN:C_TRIN + 128]
    tric_f = cst[:, C_TRIC:C_TRIC + 128]
    swab = cst[:, C_BIAS:C_BIAS + 2048].rearrange("p (h k) -> p h k", h=8)

    pp = [sb(f"pp{l}", [128, NPP]) for l in range(L)]
    g05 = [sb(f"g05_{l}", [128, 48]) for l in range(L)]
    pb = [sb(f"pb{l}", [128, 264]) for l in range(L)]
    w2 = [sb(f"w2_{l}", [16, 128]) for l in range(L)]
    bg = [sb(f"bg_{l}", [1, 128]) for l in range(L)]
    S32 = [sb(f"S32_{l}", [128, 256]) for l in range(L)]
    Sbf = [sb(f"Sbf_{l}", [128, 256], BF16) for l in range(L)]
    utail = [sb(f"utail{l}", [128, 2, 30]) for l in range(L)]
    kprev = [sb(f"kprev{l}", [128, 2, 128], BF16) for l in range(L)]
    vprev = [sb(f"vprev{l}", [128, 128], BF16) for l in range(L)]

    def ring(name, n, shape, dt=F32):
        lst = [sb(f"{name}{i}", shape, dt) for i in range(n)]
        st = {"i": 0}

        def nxt():
            a = lst[st["i"] % n]
            st["i"] += 1
            return a
        return nxt

    r_sg = ring("sg", 2, [128, 512])
    r_sd = ring("sd", 2, [128, 512])

    banks = [nc.alloc_psum_tensor(f"bank{i}", [128, 512], F32).ap() for i in range(8)]
    bstate = {"i": 0}

    def bank():
        b = banks[bstate["i"] % 8]
        bstate["i"] += 1
        return b

    def mm(out, lhsT, rhs, start=True, stop=True):
        P.op("pe", lambda h: h.matmul(out, lhsT=lhsT, rhs=rhs, start=start, stop=stop),
             reads=[lhsT, rhs], writes=[out])

    def tr(out, in_, ident):
        P.op("pe", lambda h: h.transpose(out, in_, ident), reads=[in_, ident], writes=[out])

    def act(out, in_, func, bias=None, scale=None, accum=None, eng="act", rd=None, wr=None):
        kw = {}
        rds = [in_] if rd is None else list(rd)
        if bias is not None:
            kw["bias"] = bias
            if not isinstance(bias, (int, float)):
                rds.append(bias)
        if scale is not None:
            kw["scale"] = scale
            if not isinstance(scale, (int, float)):
                rds.append(scale)
        wrs = [out] if wr is None else list(wr)
        if accum is not None:
            kw["accum_out"] = accum
            wrs.append(accum)
        P.op("act", lambda h: h.activation(out=out, in_=in_, func=func, **kw), reads=rds, writes=wrs)

    def tt(eng, out, a, b, op, rd=None, wr=None):
        P.op(eng, lambda h: h.tensor_tensor(out=out, in0=a, in1=b, op=op),
             reads=[a, b] if rd is None else rd, writes=[out] if wr is None else wr)

    def ts(eng, out, a, s1, op0, s2=None, op1=None, rd=None, wr=None):
        rds = [a] if rd is None else list(rd)
        for s in (s1, s2):
            if s is not None and not isinstance(s, (int, float)):
                rds.append(s)
        if op1 is None:
            P.op(eng, lambda h: h.tensor_scalar(out=out, in0=a, scalar1=s1, scalar2=None, op0=op0),
                 reads=rds, writes=[out] if wr is None else wr)
        else:
            P.op(eng, lambda h: h.tensor_scalar(out=out, in0=a, scalar1=s1, scalar2=s2, op0=op0, op1=op1),
                 reads=rds, writes=[out] if wr is None else wr)

    def stt(eng, out, a, s, b, op0, op1, rd=None, wr=None):
        rds = [a, b] if rd is None else list(rd)
        if not isinstance(s, (int, float)):
            rds.append(s)
        P.op(eng, lambda h: h.scalar_tensor_tensor(out=out, in0=a, scalar=s, in1=b, op0=op0, op1=op1),
             reads=rds, writes=[out] if wr is None else wr)

    def cp(eng, out, in_, rd=None, wr=None):
        if eng == "act":
            P.op("act", lambda h: h.copy(out=out, in_=in_), reads=[in_] if rd is None else rd,
                 writes=[out] if wr is None else wr)
        else:
            P.op(eng, lambda h: h.tensor_copy(out=out, in_=in_), reads=[in_] if rd is None else rd,
                 writes=[out] if wr is None else wr)

    def recip(out, in_):
        P.op("dve", lambda h: h.reciprocal(out=out, in_=in_), reads=[in_], writes=[out])

    def memset(eng, ap, v, wr=None):
        P.op(eng, lambda h: h.memset(ap, v), writes=[ap] if wr is None else wr)

    def dma(out, in_, chan, rd=None, wr=None, group=False):
        P.op("sp", lambda h: h.dma_start(out=out, in_=in_), reads=[in_] if rd is None else rd,
             writes=[out] if wr is None else wr, chan=chan, group=group)

    dma(cst, cst_d, "par", group=True)
    cp("dve", cbf, cst[:, 0:640])
    for l in range(L):
        dma(pp[l], pp_d[l], "par", group=True)
        dma(pb[l], pb_d[l].partition_broadcast(128), "par", group=True)
        dma(w2[l], w2_d[l], "par", group=True)
        dma(bg[l], bg_d[l], "par", group=True)
        ts("dve", g05[l], pp[l][:, PP_G:PP_G + 48], 0.5, ALU.mult)
        memset("dve", S32[l], 0.0)
        memset("dve", Sbf[l], 0.0)
        memset("dve", utail[l], 0.0)
        memset("dve", kprev[l], 0.0)
        memset("dve", vprev[l], 0.0)

    qblk = sb("qblk", [128, 4, 128], BF16)
    memset("dve", qblk, 0.0)

    wst = {"slot": 0, "stage": 0, "cast": 0, "pending": []}

    def load_unit(d32, dbf, n, first):
        si = wst["slot"] % NW
        wst["slot"] += 1
        slot = wslots[si]
        if first:
            off = 0
            while off < n:
                m = min(2048, n - off)
                gi = wst["stage"] % NST
                wst["stage"] += 1
                stg = stages[gi]
                dma(stg[:, 0:m], d32[:, off:off + m], f"stg{gi}")
                ce = POOL if (wst["cast"] % 3) != 2 else "act"
                wst["cast"] += 1
                cp(ce, slot[:, off:off + m], stg[:, 0:m])
                off += m
            pend = wst["pending"]
            wst["pending"] = [(slot, dbf, n, si)]
            for (s_, d_, n_, si_) in pend:
                dma(d_, s_[:, 0:n_], f"wst{si_}")
        else:
            dma(slot[:, 0:n], dbf, f"wld{si}")
        return slot

    def flush_stores():
        for (s_, d_, n_, si_) in wst["pending"]:
            dma(d_, s_[:, 0:n_], f"wst{si_}")
        wst["pending"] = []

    def rstd_from_sq(sqv, tc, n_feat_chunks, inv_n, eps):
        b = bank()
        for c in range(n_feat_chunks):
            mm(b, ones_b, sqv[:, c, tc * 512:(tc + 1) * 512], c == 0, c == n_feat_chunks - 1)
        sd = r_sd()
        act(sd, b, AF.Sqrt, bias=eps, scale=inv_n)
        recip(sd, sd)
        return sd

    def prenorm(l, n):
        for tc in range(NTC):
            sl = slice(tc * 512, (tc + 1) * 512)
            for c in range(KD):
                act(sqpre[:, c, sl], X[:, c, sl], AF.Square)
            rs = rstd_from_sq(sqpre, tc, KD, 1.0 / D, 1e-6)
            for c in range(KD):
                stt("dve", xn[:, c, sl], X[:, c, sl], pp[l][:, PP_G + n * 8 + c:PP_G + n * 8 + c + 1], rs,
                    ALU.mult, ALU.mult)

    def postnorm(l, n, tc, half_coef):
        sl = slice(tc * 512, (tc + 1) * 512)
        rs = rstd_from_sq(xn, tc, KD, 1.0 / D, 1e-6)
        gsrc = g05[l] if half_coef else pp[l]
        for c in range(KD):
            stt("dve", hb[:, c, sl], hb[:, c, sl], gsrc[:, n * 8 + c:n * 8 + c + 1], rs, ALU.mult, ALU.mult)
            tt(POOL, X[:, c, sl], X[:, c, sl], hb[:, c, sl], ALU.add)

    STOP = os.environ.get("K_STOP", "")

    def ffn(l, j, first):
        prenorm(l, 0 if j == 0 else 4)
        if STOP == "pre":
            return
        for f in range(KF):
            slot = load_unit(wgu_d[l, j, f], wgu_b[l, j, f], 2048, first)
            wv = slot[:, 0:2048].rearrange("p (g c n) -> p g c n", g=2, c=KD)
            for tc in range(NTC):
                sl = slice(tc * 512, (tc + 1) * 512)
                G = bank()
                Ub_ = bank()
                for k in range(KD):
                    mm(G, wv[:, 0, k, :], xn[:, k, sl], k == 0, k == KD - 1)
                for k in range(KD):
                    mm(Ub_, wv[:, 1, k, :], xn[:, k, sl], k == 0, k == KD - 1)
                sg = r_sg()
                act(sg, G, AF.Silu)
                tt("dve", hT[:, f, sl], sg, Ub_, ALU.mult)
        if STOP == "gu":
            return
        for dc in range(KD):
            slot = load_unit(wd_d[l, j, dc], wd_b[l, j, dc], KF * 128, first)
            wv = slot[:, 0:KF * 128].rearrange("p (f n) -> p f n", f=KF)
            for tc in range(NTC):
                sl = slice(tc * 512, (tc + 1) * 512)
                Y = bank()
                KX = os.environ.get("K_X", "")
                if KX == "loadonly":
                    continue
                for f in range(KF if KX != "mm1" else 1):
                    mm(Y, wv[:, f, :], hT[:, f, sl], f == 0, f == (KF - 1 if KX != "mm1" else 0))
                if KX == "noevac":
                    continue
                cp("dve", hb[:, dc, sl], Y)
                act(xn[:, dc, sl], hb[:, dc, sl], AF.Square)
        if STOP == "down":
            return
        for tc in range(NTC):
            postnorm(l, 1 if j == 0 else 5, tc, True)

    mo = {"o": 0}

    def carve(nwords, shape, dt=F32, parts=128):
        o = mo["o"]
        mo["o"] += nwords
        assert mo["o"] <= 11264, mo["o"]
        if dt == F32:
            v = U[0:parts, o:o + nwords]
        else:
            v = Ub[0:parts, 2 * o:2 * o + 2 * nwords]
        return v, shape

    def view(v_shape, pattern=None, **kw):
        v, shape = v_shape
        if pattern is None:
            return v
        return v.rearrange(pattern, **kw)

    gqT = view(carve(512, None))
    gkT = view(carve(512, None))
    glrT = view(carve(512, None, parts=16))
    uext = view(carve(2 * 542, None), "p (c t) -> p c t", c=2)
    cacc = view(carve(1024, None), "p (c t) -> p c t", c=2)
    sig = view(carve(512, None))
    sqT = view(carve(1024, None, BF16), "p (c t) -> p c t", c=4)
    skT = view(carve(640, None, BF16), "p (v t) -> p v t", v=2)
    gkt = view(carve(512, None), "p (s d) -> p s d", s=4)
    gv = view(carve(512, None, BF16), "p (s d) -> p s d", s=4)
    grs = view(carve(1024, None), "p (s d) -> p s d", s=4)
    sv = view(carve(320, None, BF16), "p (s d) -> p s d", s=5)
    mixg = view(carve(512, None, BF16), "p (c t) -> p c t", c=2)
    mixc = view(carve(512, None, BF16), "p (c t) -> p c t", c=2)
    mixs = view(carve(2048, None, BF16, parts=64), "p (c t) -> p c t", c=8)
    csq = sig.bitcast(BF16).rearrange("p (c t) -> p c t", c=2)

    r_lp = ring("lp", 2, [128, 128])
    r_eb = ring("eb", 2, [128, 128])
    r_enb = ring("enb", 2, [128, 128])
    r_ec = ring("ec", 2, [128, 128])
    r_kt = ring("kt", 2, [128, 128], BF16)
    r_kh = ring("kh", 2, [128, 128], BF16)
    r_atm = ring("atm", 2, [128, 512], BF16)
    r_osb = ring("osb", 2, [128, 256])
    r_osq = ring("osq", 1, [128, 256])
    r_st4 = ring("st4", 4, [128, 4])
    r_og = ring("og", 2, [128, 256], BF16)
    r_s = ring("s", 2, [128, 2, 256])
    r_pb = ring("pbf", 2, [128, 4, 256], BF16)
    r_pT = ring("pT", 2, [128, 2, 512], BF16)
    r_st2 = ring("st2", 8, [128, 2])
    r_mu = ring("mu", 1, [128, 512])
    r_t1 = ring("t1", 2, [128, 512])

    def mixer_half(l, t, half, first):
        tok0 = half * 512
        ppl = pp[l]
        very_first = (t == 0 and half == 0)
        cp("dve", uext[:, :, 0:30], utail[l])
        cp("dve", skT[:, :, 0:128], kprev[l])
        cp("dve", sv[:, 0, :], vprev[l])

        def fm_chunk(wv, gi, ncols=128):
            b = bank()
            for k in range(KD):
                mm(b[0:ncols, :], wv[:, gi, k, 0:ncols], xn[:, k, tok0:tok0 + 512], k == 0, k == KD - 1)
            return b

        units = {}

        def unit(un):
            slot = load_unit(win_d[l, un], win_b[l, un], 2048, first)
            return slot

        u0 = unit(0)[:, 0:2048].rearrange("p (g c n) -> p g c n", g=2, c=KD)
        b = fm_chunk(u0, 0)
        cp("act", gqT, b)
        b = fm_chunk(u0, 1)
        cp("act", gkT, b)
        u6 = unit(6)[:, 0:2048].rearrange("p (g c n) -> p g c n", g=2, c=KD)
        b = fm_chunk(u6, 0, 16)
        cp("act", glrT, b[0:16, :])
        u2 = unit(2)[:, 0:2048].rearrange("p (g c n) -> p g c n", g=2, c=KD)
        u1 = unit(1)[:, 0:2048].rearrange("p (g c n) -> p g c n", g=2, c=KD)
        for c in range(2):
            b = fm_chunk(u2, c)
            act(sig, b, AF.Sigmoid)
            b2 = fm_chunk(u1, c)
            tt("dve", uext[:, c, 30:542], b2, sig, ALU.mult)
        for un in (3, 4):
            uu = unit(un)[:, 0:2048].rearrange("p (g c n) -> p g c n", g=2, c=KD)
            for gi in range(2):
                b = fm_chunk(uu, gi)
                cp("act", sqT[:, (un - 3) * 2 + gi, :], b)
        u5 = unit(5)[:, 0:2048].rearrange("p (g c n) -> p g c n", g=2, c=KD)
        for kv in range(2):
            b = fm_chunk(u5, kv)
            cp("dve", skT[:, kv, 128:640], b)
        tmu = [unit(7 + i)[:, 0:2048].rearrange("p (c n) -> p c n", c=KD) for i in range(3)]
        for s in range(4):
            ts_ = slice(tok0 + s * 128, tok0 + (s + 1) * 128)
            bs = []
            for i in range(3):
                b = bank()
                for k in range(KD):
                    mm(b[:, 0:256], xn[:, k, ts_], tmu[i][:, k, :], k == 0, k == KD - 1)
                bs.append(b)
            cp("dve", gkt[:, s, :], bs[0][:, 0:128])
            cp("act", sv[:, 1 + s, :], bs[0][:, 128:256])
            cp("act", gv[:, s, :], bs[1][:, 0:256])
            act(grs[:, s, :], bs[2][:, 0:256], AF.Silu)
        for s in range(4):
            tt("pool", grs[:, s, :], grs[:, s, :], pb[l][:, 0:256], ALU.mult)

        MS = os.environ.get("K_MSTOP", "")
        if MS == "proj":
            return
        for c in range(2):
            eng = "dve"
            cw0 = PP_CW + c * 31
            ts(eng, cacc[:, c, :], uext[:, c, 0:512], ppl[:, cw0:cw0 + 1], ALU.mult,
               ppl[:, PP_CB + c:PP_CB + c + 1], ALU.add)
            for w in range(1, 31):
                stt(eng, cacc[:, c, :], uext[:, c, w:w + 512], ppl[:, cw0 + w:cw0 + w + 1], cacc[:, c, :],
                    ALU.mult, ALU.add)
            cp(eng, utail[l][:, c, :], uext[:, c, 512:542])

        if MS == "conv":
            return
        for s in range(4):
            ssl = slice(s * 128, (s + 1) * 128)
            zb = bank()
            mm(zb[:, 0:128], glrT[0:16, ssl], w2[l], True, False)
            mm(zb[:, 0:128], ones_f[0:1, 0:128], bg[l], False, True)
            lp = r_lp()
            act(lp, zb[:, 0:128], AF.Exp, scale=-1.0)
            act(lp, lp, AF.Ln, bias=1.0)
            bb = bank()
            mm(bb[:, 0:128], lp, trin_f, True, True)
            mm(bb[:, 128:256], tric_f, lp, True, True)
            eb = r_eb()
            act(eb, bb[:, 0:128], AF.Exp)
            enb = r_enb()
            act(enb, bb[:, 0:128], AF.Exp, scale=-1.0)
            ec = r_ec()
            act(ec, bb[:, 128:256], AF.Exp)
            for hh in range(4):
                ps = slice(32 * hh, 32 * hh + 32)
                stt("dve", qblk[ps, hh, :], gqT[ps, ssl], 32.0 ** -0.5, eb[ps, :], ALU.mult, ALU.mult)
            kt = r_kt()
            tt("dve", kt, gkT[:, ssl], enb, ALU.mult)
            kh = r_kh()
            tt("pool", kh, gkt[:, s, :], ec, ALU.mult)
            ab = bank()
            mm(ab, kt, qblk.rearrange("p h i -> p (h i)"), True, True)
            atm = r_atm()
            tt("dve", atm.rearrange("p (h i) -> p h i", h=4), ab.rearrange("p (h i) -> p h i", h=4),
               mask_b.unsqueeze(1).broadcast_to([128, 4, 128]), ALU.mult,
               rd=[ab, mask_b], wr=[atm])
            ob = bank()
            for hh in range(4):
                mm(ob[:, hh * 64:(hh + 1) * 64], atm[:, hh * 128:(hh + 1) * 128], gv[:, s, hh * 64:(hh + 1) * 64],
                   True, False)
                mm(ob[:, hh * 64:(hh + 1) * 64], qblk[:, hh, :], Sbf[l][:, hh * 64:(hh + 1) * 64], False, True)
            ub = bank()
            mm(ub[:, 0:256], kh, gv[:, s, :], True, True)
            stt("dve", S32[l], S32[l], eb[:, 127:128], ub[:, 0:256], ALU.mult, ALU.add)
            cp("pool", Sbf[l], S32[l])
            osb = r_osb()
            cp("act", osb, ob[:, 0:256])
            osq = r_osq()
            tt("pool", osq, osb, osb, ALU.mult)
            ssq = r_st4()
            P.op("dve", lambda h, ssq=ssq, osq=osq: h.reduce_sum(
                out=ssq, in_=osq.rearrange("p (h d) -> p h d", h=4), axis=AX.X),
                reads=[osq], writes=[ssq])
            lnv = r_st4()
            act(lnv, ssq, AF.Ln, bias=1e-6, scale=1.0 / 64.0)
            rn = r_st4()
            act(rn, lnv, AF.Exp, scale=-0.5)
            tt("dve", osb.rearrange("p (h d) -> p h d", h=4), osb.rearrange("p (h d) -> p h d", h=4),
               rn.unsqueeze(2).broadcast_to([128, 4, 64]), ALU.mult, rd=[osb, rn], wr=[osb])
            og = r_og()
            tt("dve", og, osb, grs[:, s, :], ALU.mult)
            tb = bank().bitcast(BF16)
            for c in range(2):
                tr(tb[:, c * 128:(c + 1) * 128], og[:, c * 128:(c + 1) * 128], ident_b)
            cp("act", mixg[:, :, ssl], tb[:, 0:256].rearrange("p (c t) -> p c t", c=2),
               rd=[tb], wr=[mixg[:, 0, ssl], mixg[:, 1, ssl]])

        if MS == "gla":
            return
        for n in range(4):
            qsl = slice(n * 128, (n + 1) * 128)
            for kv in range(2):
                pbf = r_pb()
                for pr in range(2):
                    h0 = kv * 4 + pr * 2
                    sbk = bank()
                    for gi in range(2):
                        hh = kv * 4 + pr + 2 * gi
                        po = (hh % 2) * 64
                        mm(sbk[:, gi * 256:(gi + 1) * 256], sqT[po:po + 64, hh // 2, qsl],
                           skT[po:po + 64, kv, n * 128:n * 128 + 256], True, True)
                    s_ = r_s()
                    stt("dve", s_, sbk.rearrange("p (g k) -> p g k", g=2), 0.125, swab[:, h0:h0 + 2, :],
                        ALU.mult, ALU.add, rd=[sbk, swab[:, h0, :], swab[:, h0 + 1, :]], wr=[s_])
                    if very_first and n == 0:
                        memset("dve", s_[:, :, 0:128], NEG, wr=[s_])
                    m = r_st2()
                    P.op("dve", lambda h, m=m, s_=s_: h.reduce_max(out=m, in_=s_, axis=AX.X),
                         reads=[s_], writes=[m])
                    tt("dve", m, m, pb[l][:, 256 + h0:256 + h0 + 2], ALU.max)
                    negm = r_st2()
                    ts("dve", negm, m, -1.0, ALU.mult)
                    dsk = r_st2()
                    tt("dve", dsk, pb[l][:, 256 + h0:256 + h0 + 2], m, ALU.subtract)
                    esk = r_st2()
                    act(esk, dsk, AF.Exp)
                    p_ = s_
                    rsum = r_st2()
                    for gi in range(2):
                        act(p_[:, gi, :], s_[:, gi, :], AF.Exp, bias=negm[:, gi:gi + 1], rd=[s_], wr=[p_])
                    P.op("dve", lambda h, rsum=rsum, p_=p_: h.reduce_sum(out=rsum, in_=p_, axis=AX.X),
                         reads=[p_], writes=[rsum])
                    den = r_st2()
                    tt("dve", den, rsum, esk, ALU.add)
                    rden = r_st2()
                    recip(rden, den)
                    for gi in range(2):
                        ts("dve", pbf[:, pr + 2 * gi, :], p_[:, gi, :], rden[:, gi:gi + 1], ALU.mult,
                           rd=[p_], wr=[pbf])
                tb = bank().bitcast(BF16)
                for kb in range(2):
                    for g in range(4):
                        tr(tb[:, kb * 512 + g * 128:kb * 512 + (g + 1) * 128],
                           pbf[:, g, kb * 128:(kb + 1) * 128], ident_b)
                pT = r_pT()
                cp("act", pT.rearrange("p k q -> p (k q)"), tb, rd=[tb], wr=[pT])
                ob = bank()
                for kb in range(2):
                    mm(ob[0:64, :], sv[:, n + kb, kv * 64:(kv + 1) * 64], pT[:, kb, :], kb == 0, kb == 1)
                cp("dve", mixs[:, kv * 4:(kv + 1) * 4, qsl], ob[0:64, :].rearrange("p (g q) -> p g q", g=4),
                   rd=[ob], wr=[mixs[:, kv * 4 + g, qsl] for g in range(4)])
        cp("dve", kprev[l], skT[:, :, 512:640])
        cp("dve", vprev[l], sv[:, 4, :])

        if MS == "swa":
            return
        for c in range(2):
            act(csq[:, c, :], cacc[:, c, :], AF.Square)
        mb = bank()
        for c in range(2):
            mm(mb, ones_f, cacc[:, c, :], c == 0, c == 1)
        qb = bank()
        for c in range(2):
            mm(qb, ones_b, csq[:, c, :], c == 0, c == 1)
        mu = r_mu()
        ts("dve", mu, mb, 1.0 / 256.0, ALU.mult)
        musq = r_t1()
        tt("pool", musq, mu, mu, ALU.mult)
        var = r_t1()
        stt("dve", var, qb, 1.0 / 256.0, musq, ALU.mult, ALU.subtract)
        rs = r_sd()
        act(rs, var, AF.Sqrt, bias=1e-5)
        recip(rs, rs)
        for c in range(2):
            t1 = r_t1()
            tt("dve", t1, cacc[:, c, :], mu, ALU.subtract)
            tt("dve", t1, t1, rs, ALU.mult)
            act(mixc[:, c, :], t1, AF.Silu, bias=ppl[:, PP_LB + c:PP_LB + c + 1], scale=ppl[:, PP_LG + c:PP_LG + c + 1])

        if MS == "ln":
            return
        for dc in range(KD):
            slot = load_unit(wo_d[l, dc], wo_b[l, dc], 1536, first)
            wv = slot[:, 0:1536].rearrange("p (k n) -> p k n", k=12)
            Y = bank()
            for kc in range(12):
                if kc < 2:
                    mm(Y, wv[:, kc, :], mixg[:, kc, :], kc == 0, False)
                elif kc < 4:
                    mm(Y, wv[:, kc, :], mixc[:, kc - 2, :], False, False)
                else:
                    mm(Y, wv[0:64, kc, :], mixs[:, kc - 4, :], False, kc == 11)
            sl = slice(tok0, tok0 + 512)
            cp("dve", hb[:, dc, sl], Y)
            act(xn[:, dc, sl], hb[:, dc, sl], AF.Square)
        postnorm(l, 3, half, False)

    for t in range(NT):
        first = (t == 0)
        for c in range(KD):
            dma(X[:, c, :], xT[c * 128:(c + 1) * 128, t * T:(t + 1) * T], "xin")
        for l in range(L):
            if dbg == ("init", l):
                break
            ffn(l, 0, first)
            if dbg == ("ffn1", l):
                break
            prenorm(l, 2)
            for half in range(NTC):
                mixer_half(l, t, half, first)
            if dbg == ("mix", l):
                break
            ffn(l, 1, first)
        if first:
            flush_stores()
        for c in range(KD):
            dma(outT[c * 128:(c + 1) * 128, t * T:(t + 1) * T], X[:, c, :], "xout")

    with nc.allow_low_precision("bf16 matmuls with fp32 accumulation"):
        P.emit(final_wait_chans=["xout"])
    return nc, P


_CACHE = {}


def _run(inp, S, L, n_cores, dbg=None, trace=False):
    key = (S, L, dbg)
    if key not in _CACHE:
        _CACHE[key] = build(S, L, dbg=dbg)
    nc, _ = _CACHE[key]
    wts = _prep_weights(inp, L)
    cst = _const_table()
    x = np.asarray(inp["x"], np.float32)
    in_maps = []
    for b in range(n_cores):
        m = dict(wts)
        m["cst"] = cst
        m["xT"] = np.ascontiguousarray(x[b].T)
        in_maps.append(m)
    res = run_bass_kernel_spmd(nc, in_maps, core_ids=list(range(n_cores)), trace=trace)
    out = np.stack([np.ascontiguousarray(r["outT"].T) for r in res.results], 0)
    return out, res


def kernel(x, norm_g, ffn_w_gate, ffn_w_up, ffn_w_down, w_in, gla_w_gate2, gla_b_gate,
           gla_norm_g, conv_w, conv_b, conv_ln_g, conv_ln_b, swa_sinks, w_out):
    inp = dict(x=x, norm_g=norm_g, ffn_w_gate=ffn_w_gate, ffn_w_up=ffn_w_up, ffn_w_down=ffn_w_down,
               w_in=w_in, gla_w_gate2=gla_w_gate2, gla_b_gate=gla_b_gate, gla_norm_g=gla_norm_g,
               conv_w=conv_w, conv_b=conv_b, conv_ln_g=conv_ln_g, conv_ln_b=conv_ln_b,
               swa_sinks=swa_sinks, w_out=w_out)
    inp = {k: np.asarray(v, np.float32) for k, v in inp.items()}
    B, S, _ = inp["x"].shape
    L = inp["norm_g"].shape[0]
    out, _ = _run(inp, S, L, B)
    return out.astype(np.float32)
```

```python
import contextlib
import numpy as np
import concourse.bass as bass
import concourse.mybir as mybir
from concourse.bass_utils import run_bass_kernel_spmd

F32 = mybir.dt.float32
BF16 = mybir.dt.bfloat16
AF = mybir.ActivationFunctionType
ALU = mybir.AluOpType
AX = mybir.AxisListType

_DSZ = {F32: 4, BF16: 2}

D = 1024
KD = 8
FF = 2816
KF = 22
NEG = -30000.0


def _region(ap):
    t = ap.tensor
    kind = type(t).__name__
    dims = ap.ap
    off = int(ap.offset)
    esz = _DSZ[ap.dtype]
    if kind.startswith("DRam"):
        ext = sum((c - 1) * abs(s) for s, c in dims) + 1
        return (t.name, 0, 1, off * esz, (off + ext) * esz)
    pstep, npart = dims[0]
    if pstep <= 0:
        p_lo, p_hi, fo = 0, 128, off
    else:
        p_lo = off // pstep
        p_hi = p_lo + npart
        fo = off % pstep
    if kind.startswith("PSum"):
        return (t.name, 0, 128, 0, 1 << 30)
    ext = sum((c - 1) * abs(s) for s, c in dims[1:]) + 1
    return (t.name, p_lo, p_hi, fo * esz, (fo + ext) * esz)


def _overlap(a, b):
    return a[1] < b[2] and b[1] < a[2] and a[3] < b[4] and b[3] < a[4]


def _covers(a, b):
    return a[1] <= b[1] and a[2] >= b[2] and a[3] <= b[3] and a[4] >= b[4]


class Prog:
    ENGS = ("pe", "act", "dve", "pool", "sp")

    def __init__(self, nc):
        self.nc = nc
        self.ops = {e: [] for e in self.ENGS}
        self.wrec = {}
        self.rrec = {}
        self.dma_cnt = {}
        self.same_eng_sync = {"act", "dve", "pool"}

    def op(self, eng, fn, reads=(), writes=(), chan=None, group=False):
        deps_e = {}
        deps_d = {}

        def add(tok):
            if tok[0] == "e":
                _, e, i = tok
                if e == eng and e not in self.same_eng_sync:
                    return
                if deps_e.get(e, -1) < i:
                    deps_e[e] = i
            else:
                _, c, n = tok
                if deps_d.get(c, 0) < n:
                    deps_d[c] = n

        rregs = [_region(a) for a in reads]
        wregs = [_region(a) for a in writes]
        for r in rregs:
            for (reg, tok) in self.wrec.get(r[0], ()):
                if _overlap(reg, r):
                    add(tok)
            if r[0].startswith("bank"):
                for (reg, tok) in self.rrec.get(r[0], ()):
                    if tok[0] == "e" and tok[1] != eng:
                        add(tok)
        for w in wregs:
            for (reg, tok) in self.wrec.get(w[0], ()):
                if _overlap(reg, w):
                    add(tok)
            for (reg, tok) in self.rrec.get(w[0], ()):
                if _overlap(reg, w):
                    add(tok)
        idx = len(self.ops[eng])
        if chan is None:
            tok = ("e", eng, idx)
        else:
            n = self.dma_cnt.get(chan, 0) + 1
            self.dma_cnt[chan] = n
            tok = ("d", chan, (1 << 30) if group else n)
        for r in rregs:
            lst = self.rrec.setdefault(r[0], [])
            if tok[0] == "e":
                lst[:] = [(g, t) for (g, t) in lst
                          if not (t[0] == "e" and t[1] == eng and _covers(r, g))]
            lst.append((r, tok))
        for w in wregs:
            wl = self.wrec.setdefault(w[0], [])
            wl[:] = [(g, t) for (g, t) in wl if not _covers(w, g)]
            wl.append((w, tok))
            rl = self.rrec.get(w[0])
            if rl:
                rl[:] = [(g, t) for (g, t) in rl if not _covers(w, g)]
        self.ops[eng].append(dict(fn=fn, de=deps_e, dd=deps_d, chan=chan, sig=False))
        return tok

    def emit(self, final_wait_chans=()):
        nc = self.nc
        ops = self.ops
        for e in self.ENGS:
            for o in ops[e]:
                for (f, i) in o["de"].items():
                    ops[f][i]["sig"] = True
        cum = {}
        for e in self.ENGS:
            c = 0
            arr = []
            for o in ops[e]:
                if o["sig"]:
                    c += 1
                arr.append(c)
            cum[e] = arr
        chans = sorted(self.dma_cnt.keys())
        with contextlib.ExitStack() as st:
            esem = {e: st.enter_context(nc.semaphore("s_" + e)) for e in self.ENGS}
            dsem = {c: st.enter_context(nc.semaphore("d_" + str(c))) for c in chans}
            block = st.enter_context(nc.Block())

            def run(e, h):
                waited_e = {}
                waited_d = {}
                for o in ops[e]:
                    for (f, i) in o["de"].items():
                        tgt = cum[f][i]
                        if waited_e.get(f, 0) < tgt:
                            h.wait_ge(esem[f], tgt)
                            waited_e[f] = tgt
                    for (c, n) in o["dd"].items():
                        n = min(n, self.dma_cnt[c])
                        if waited_d.get(c, 0) < n:
                            h.wait_ge(dsem[c], 16 * n)
                            waited_d[c] = n
                    ins = o["fn"](h)
                    if o["chan"] is not None:
                        ins.then_inc(dsem[o["chan"]], 16)
                    if o["sig"]:
                        ins.then_inc(esem[e], 1)
                if e == "sp":
                    for c in final_wait_chans:
                        h.wait_ge(dsem[c], 16 * self.dma_cnt[c])

            @block.tensor
            def _(h):
                run("pe", h)

            @block.scalar
            def _(h):
                run("act", h)

            @block.vector
            def _(h):
                run("dve", h)

            @block.gpsimd
            def _(h):
                run("pool", h)

            @block.sync
            def _(h):
                run("sp", h)


C_ID, C_ONE, C_TRIN, C_TRIC, C_MASK, C_BIAS = 0, 128, 256, 384, 512, 640
NCONST = 640 + 8 * 256


_HPERM = [0, 2, 1, 3, 4, 6, 5, 7]


def _const_table():
    c = np.zeros((128, NCONST), np.float32)
    j = np.arange(128)[:, None]
    i = np.arange(128)[None, :]
    c[:, C_ID:C_ID + 128] = (j == i)
    c[:, C_ONE:C_ONE + 128] = 1.0
    c[:, C_TRIN:C_TRIN + 128] = np.where(j <= i, -1.0 / 16.0, 0.0)
    c[:, C_TRIC:C_TRIC + 128] = np.where(j > i, -1.0 / 16.0, 0.0)
    c[:, C_MASK:C_MASK + 128] = (j <= i)
    q = np.arange(128)[:, None]
    kj = np.arange(256)[None, :]
    dist = q - kj + 128
    valid = (dist >= 0) & (dist < 128)
    for jpos, h in enumerate(_HPERM):
        slope = 2.0 ** (-(h + 1))
        c[:, C_BIAS + jpos * 256:C_BIAS + (jpos + 1) * 256] = np.where(valid, -slope * dist, NEG)
    return c


_O_GQ, _O_GK, _O_GV, _O_GR, _O_GLR, _O_CA, _O_CG, _O_SQ, _O_SK, _O_SV = (
    0, 128, 256, 512, 768, 784, 1040, 1296, 1808, 1936)

PP_G, PP_CW, PP_CB, PP_LG, PP_LB, NPP = 0, 48, 110, 112, 114, 116


def _fm(v, nch):
    return np.ascontiguousarray(v.reshape(nch, 128).T)


def _prep_weights(inp, L):
    f32 = np.float32
    wgu = np.empty((L, 2, KF, 128, 2048), f32)
    wd = np.empty((L, 2, KD, 128, KF * 128), f32)
    win = np.zeros((L, 10, 128, 2048), f32)
    wo = np.zeros((L, KD, 128, 12 * 128), f32)
    pp = np.zeros((L, 128, NPP), f32)
    pb = np.zeros((L, 264), f32)
    w2 = np.ascontiguousarray(inp["gla_w_gate2"][:L]).astype(f32)
    bg = np.ascontiguousarray(inp["gla_b_gate"][:L]).astype(f32).reshape(L, 1, 128)
    for l in range(L):
        for j in range(2):
            g = inp["ffn_w_gate"][l, j].reshape(KD, 128, KF, 128)
            u = inp["ffn_w_up"][l, j].reshape(KD, 128, KF, 128)
            wgu[l, j] = np.stack([g, u], 0).transpose(3, 2, 0, 1, 4).reshape(KF, 128, 2048)
            dn = inp["ffn_w_down"][l, j].reshape(KF, 128, KD, 128)
            wd[l, j] = dn.transpose(2, 1, 0, 3).reshape(KD, 128, KF * 128)
        w = inp["w_in"][l]
        cols = [w[:, _O_GQ:_O_GQ + 128], w[:, _O_GK:_O_GK + 128]]
        cols += [w[:, _O_CA + 128 * i:_O_CA + 128 * (i + 1)] for i in range(2)]
        cols += [w[:, _O_CG + 128 * i:_O_CG + 128 * (i + 1)] for i in range(2)]
        cols += [w[:, _O_SQ + 128 * i:_O_SQ + 128 * (i + 1)] for i in range(4)]
        for kv in range(2):
            k = w[:, _O_SK + 64 * kv:_O_SK + 64 * (kv + 1)]
            cols.append(np.concatenate([k, k], 1))
        glr = np.zeros((D, 128), f32)
        glr[:, :16] = w[:, _O_GLR:_O_GLR + 16]
        cols.append(glr)
        cols.append(np.zeros((D, 128), f32))
        for un in range(7):
            a = cols[2 * un].reshape(KD, 128, 128)
            b = cols[2 * un + 1].reshape(KD, 128, 128)
            win[l, un] = np.stack([a, b], 0).transpose(2, 0, 1, 3).reshape(128, 2048)
        tm = [np.concatenate([w[:, _O_GK:_O_GK + 128], w[:, _O_SV:_O_SV + 128]], 1),
              w[:, _O_GV:_O_GV + 256], w[:, _O_GR:_O_GR + 256]]
        for un in range(3):
            win[l, 7 + un] = tm[un].reshape(KD, 128, 256).transpose(1, 0, 2).reshape(128, 2048)
        o = inp["w_out"][l]
        for dc in range(KD):
            blk = np.zeros((128, 12, 128), f32)
            oc = o[:, dc * 128:(dc + 1) * 128]
            blk[:, 0:4, :] = oc[0:512].reshape(4, 128, 128).transpose(1, 0, 2)
            blk[0:64, 4:12, :] = oc[512:1024].reshape(8, 64, 128).transpose(1, 0, 2)
            wo[l, dc] = blk.reshape(128, 1536)
        pp[l, :, PP_G:PP_G + 48] = inp["norm_g"][l].reshape(6, KD, 128).transpose(2, 0, 1).reshape(128, 48)
        pp[l, :, PP_CW:PP_CW + 62] = inp["conv_w"][l].reshape(31, 2, 128).transpose(2, 1, 0).reshape(128, 62)
        pp[l, :, PP_CB:PP_CB + 2] = _fm(inp["conv_b"][l], 2)
        pp[l, :, PP_LG:PP_LG + 2] = _fm(inp["conv_ln_g"][l], 2)
        pp[l, :, PP_LB:PP_LB + 2] = _fm(inp["conv_ln_b"][l], 2)
        pb[l, 0:256] = inp["gla_norm_g"][l].reshape(256)
        pb[l, 256:264] = inp["swa_sinks"][l][_HPERM]
    return dict(wgu=wgu, wd=wd, win=win, wo=wo, pp=pp, pb=pb.reshape(L, 1, 264), w2=w2, bg=bg)


import os
POOL = os.environ.get("K_POOL", "pool")


def build(S, L, T=512, dbg=None):
    assert S % T == 0 and T % 512 == 0
    NT = S // T
    NTC = T // 512
    nc = bass.Bass("TRN2", target_bir_lowering=False)
    P = Prog(nc)

    def dram(name, shape, dt=F32, kind="ExternalInput"):
        return nc.dram_tensor(name, list(shape), dt, kind=kind).ap()

    xT = dram("xT", [D, S])
    outT = dram("outT", [D, S], kind="ExternalOutput")
    cst_d = dram("cst", [128, NCONST])
    pp_d = dram("pp", [L, 128, NPP])
    pb_d = dram("pb", [L, 1, 264])
    w2_d = dram("w2", [L, 16, 128])
    bg_d = dram("bg", [L, 1, 128])
    wgu_d = dram("wgu", [L, 2, KF, 128, 2048])
    wd_d = dram("wd", [L, 2, KD, 128, KF * 128])
    win_d = dram("win", [L, 10, 128, 2048])
    wo_d = dram("wo", [L, KD, 128, 1536])
    wgu_b = dram("wgu_b", [L, 2, KF, 128, 2048], BF16, kind="Internal")
    wd_b = dram("wd_b", [L, 2, KD, 128, KF * 128], BF16, kind="Internal")
    win_b = dram("win_b", [L, 10, 128, 2048], BF16, kind="Internal")
    wo_b = dram("wo_b", [L, KD, 128, 1536], BF16, kind="Internal")

    def sb(name, shape, dt=F32):
        return nc.alloc_sbuf_tensor(name, list(shape), dt).ap()

    X = sb("X", [128, KD, T])
    xn = sb("xn", [128, KD, T], BF16)
    U = sb("U", [128, 15360])
    Ub = U.bitcast(BF16)
    hT = Ub[:, 0:KF * T].rearrange("p (f t) -> p f t", f=KF)
    hb = U[:, 11264:11264 + KD * T].rearrange("p (c t) -> p c t", c=KD)
    sqpre = Ub[:, 0:KD * T].rearrange("p (c t) -> p c t", c=KD)

    NW = 4
    wslots = [sb(f"wsl{i}", [128, 3072], BF16) for i in range(NW)]
    NST = 2
    stages = [sb(f"stg{i}", [128, 2048]) for i in range(NST)]
    cst = sb("cstf", [128, NCONST])
    cbf = sb("cstb", [128, 640], BF16)
    ident_b = cbf[:, C_ID:C_ID + 128]
    ones_b = cbf[:, C_ONE:C_ONE + 128]
    mask_b = cbf[:, C_MASK:C_MASK + 128]
    ones_f = cst[:, C_ONE:C_ONE + 128]
    trin_f = cst[:, C_TRIN:C_TRIN + 128]
    tric_f = cst[:, C_TRIC:C_TRIC + 128]
    swab = cst[:, C_BIAS:C_BIAS + 2048].rearrange("p (h k) -> p h k", h=8)

    pp = [sb(f"pp{l}", [128, NPP]) for l in range(L)]
    g05 = [sb(f"g05_{l}", [128, 48]) for l in range(L)]
    pb = [sb(f"pb{l}", [128, 264]) for l in range(L)]
    w2 = [sb(f"w2_{l}", [16, 128]) for l in range(L)]
    bg = [sb(f"bg_{l}", [1, 128]) for l in range(L)]
    nsink = [sb(f"nsink{l}", [128, 8]) for l in range(L)]
    S32 = [sb(f"S32_{l}", [128, 256]) for l in range(L)]
    Sbf = [sb(f"Sbf_{l}", [128, 256], BF16) for l in range(L)]
    utail = [sb(f"utail{l}", [128, 2, 30], BF16) for l in range(L)]
    kprev = [sb(f"kprev{l}", [128, 2, 128], BF16) for l in range(L)]
    vprev = [sb(f"vprev{l}", [128, 128], BF16) for l in range(L)]

    def ring(name, n, shape, dt=F32):
        lst = [sb(f"{name}{i}", shape, dt) for i in range(n)]
        st = {"i": 0}

        def nxt():
            a = lst[st["i"] % n]
            st["i"] += 1
            return a
        return nxt

    r_sg = ring("sg", 2, [128, 512])
    r_sd = ring("sd", 2, [128, 512])

    banks = [nc.alloc_psum_tensor(f"bank{i}", [128, 512], F32).ap() for i in range(8)]
    bstate = {"i": 0}

    def bank():
        b = banks[bstate["i"] % 8]
        bstate["i"] += 1
        return b

    def mm(out, lhsT, rhs, start=True, stop=True):
        P.op("pe", lambda h: h.matmul(out, lhsT=lhsT, rhs=rhs, start=start, stop=stop),
             reads=[lhsT, rhs], writes=[out])

    def tr(out, in_, ident):
        P.op("pe", lambda h: h.transpose(out, in_, ident), reads=[in_, ident], writes=[out])

    def act(out, in_, func, bias=None, scale=None, accum=None, eng="act", rd=None, wr=None):
        kw = {}
        rds = [in_] if rd is None else list(rd)
        if bias is not None:
            kw["bias"] = bias
            if not isinstance(bias, (int, float)):
                rds.append(bias)
        if scale is not None:
            kw["scale"] = scale
            if not isinstance(scale, (int, float)):
                rds.append(scale)
        wrs = [out] if wr is None else list(wr)
        if accum is not None:
            kw["accum_out"] = accum
            wrs.append(accum)
        P.op("act", lambda h: h.activation(out=out, in_=in_, func=func, **kw), reads=rds, writes=wrs)

    def tt(eng, out, a, b, op, rd=None, wr=None):
        P.op(eng, lambda h: h.tensor_tensor(out=out, in0=a, in1=b, op=op),
             reads=[a, b] if rd is None else rd, writes=[out] if wr is None else wr)

    def ts(eng, out, a, s1, op0, s2=None, op1=None, rd=None, wr=None):
        rds = [a] if rd is None else list(rd)
        for s in (s1, s2):
            if s is not None and not isinstance(s, (int, float)):
                rds.append(s)
        if op1 is None:
            P.op(eng, lambda h: h.tensor_scalar(out=out, in0=a, scalar1=s1, scalar2=None, op0=op0),
                 reads=rds, writes=[out] if wr is None else wr)
        else:
            P.op(eng, lambda h: h.tensor_scalar(out=out, in0=a, scalar1=s1, scalar2=s2, op0=op0, op1=op1),
                 reads=rds, writes=[out] if wr is None else wr)

    def stt(eng, out, a, s, b, op0, op1, rd=None, wr=None):
        rds = [a, b] if rd is None else list(rd)
        if not isinstance(s, (int, float)):
            rds.append(s)
        P.op(eng, lambda h: h.scalar_tensor_tensor(out=out, in0=a, scalar=s, in1=b, op0=op0, op1=op1),
             reads=rds, writes=[out] if wr is None else wr)

    def cp(eng, out, in_, rd=None, wr=None):
        if eng == "act":
            P.op("act", lambda h: h.copy(out=out, in_=in_), reads=[in_] if rd is None else rd,
                 writes=[out] if wr is None else wr)
        else:
            P.op(eng, lambda h: h.tensor_copy(out=out, in_=in_), reads=[in_] if rd is None else rd,
                 writes=[out] if wr is None else wr)

    def recip(out, in_):
        P.op("dve", lambda h: h.reciprocal(out=out, in_=in_), reads=[in_], writes=[out])

    def memset(eng, ap, v, wr=None):
        P.op(eng, lambda h: h.memset(ap, v), writes=[ap] if wr is None else wr)

    def dma(out, in_, chan, rd=None, wr=None, group=False):
        P.op("sp", lambda h: h.dma_start(out=out, in_=in_), reads=[in_] if rd is None else rd,
             writes=[out] if wr is None else wr, chan=chan, group=group)

    dma(cst, cst_d, "par", group=True)
    cp("dve", cbf, cst[:, 0:640])
    for l in range(L):
        dma(pp[l], pp_d[l], "par", group=True)
        dma(pb[l], pb_d[l].partition_broadcast(128), "par", group=True)
        dma(w2[l], w2_d[l], "par", group=True)
        dma(bg[l], bg_d[l], "par", group=True)
        ts("dve", g05[l], pp[l][:, PP_G:PP_G + 48], 0.5, ALU.mult)
        ts("dve", nsink[l], pb[l][:, 256:264], -1.0, ALU.mult)
        memset("dve", S32[l], 0.0)
        memset("dve", Sbf[l], 0.0)
        memset("dve", utail[l], 0.0)
        memset("dve", kprev[l], 0.0)
        memset("dve", vprev[l], 0.0)

    cdiag1 = sb("cdiag", [128, 62, 128], BF16)
    cdiag = [cdiag1 for l in range(L)]

    def build_cdiag(l):
        for k in range(62):
            tt("pool", cdiag1[:, k, :], cst[:, C_ID:C_ID + 128],
               pp[l][:, PP_CW + k:PP_CW + k + 1].broadcast_to([128, 128]), ALU.mult,
               rd=[cst[:, C_ID:C_ID + 128], pp[l][:, PP_CW + k:PP_CW + k + 1]])

    qblk = sb("qblk", [128, 4, 128], BF16)
    memset("dve", qblk, 0.0)

    wst = {"slot": 0, "stage": 0, "cast": 0, "pending": []}

    def load_unit(d32, dbf, n, first):
        si = wst["slot"] % NW
        wst["slot"] += 1
        slot = wslots[si]
        if first:
            off = 0
            while off < n:
                m = min(2048, n - off)
                gi = wst["stage"] % NST
                wst["stage"] += 1
                stg = stages[gi]
                dma(stg[:, 0:m], d32[:, off:off + m], f"stg{gi}")
                ce = POOL if (wst["cast"] % 3) == 2 else "act"
                wst["cast"] += 1
                cp(ce, slot[:, off:off + m], stg[:, 0:m])
                off += m
            pend = wst["pending"]
            wst["pending"] = [(slot, dbf, n, si)]
            for (s_, d_, n_, si_) in pend:
                dma(d_, s_[:, 0:n_], f"wst{si_}")
        else:
            dma(slot[:, 0:n], dbf, f"wld{si}")
        return slot

    def flush_stores():
        for (s_, d_, n_, si_) in wst["pending"]:
            dma(d_, s_[:, 0:n_], f"wst{si_}")
        wst["pending"] = []

    def rstd_from_sq(sqv, tc, n_feat_chunks, inv_n, eps):
        b = bank()
        for c in range(n_feat_chunks):
            mm(b, ones_b, sqv[:, c, tc * 512:(tc + 1) * 512], c == 0, c == n_feat_chunks - 1)
        sd = r_sd()
        act(sd, b, AF.Sqrt, bias=eps, scale=inv_n)
        recip(sd, sd)
        return sd

    def prenorm(l, n):
        for tc in range(NTC):
            sl = slice(tc * 512, (tc + 1) * 512)
            for c in range(KD):
                act(sqpre[:, c, sl], X[:, c, sl], AF.Square)
            rs = rstd_from_sq(sqpre, tc, KD, 1.0 / D, 1e-6)
            for c in range(KD):
                stt("dve", xn[:, c, sl], X[:, c, sl], pp[l][:, PP_G + n * 8 + c:PP_G + n * 8 + c + 1], rs,
                    ALU.mult, ALU.mult)

    def postnorm(l, n, tc, half_coef):
        sl = slice(tc * 512, (tc + 1) * 512)
        rs = rstd_from_sq(xn, tc, KD, 1.0 / D, 1e-6)
        gsrc = g05[l] if half_coef else pp[l]
        for c in range(KD):
            stt("dve", hb[:, c, sl], hb[:, c, sl], gsrc[:, n * 8 + c:n * 8 + c + 1], rs, ALU.mult, ALU.mult)
            tt("dve", X[:, c, sl], X[:, c, sl], hb[:, c, sl], ALU.add)

    STOP = os.environ.get("K_STOP", "")

    def ffn(l, j, first):
        prenorm(l, 0 if j == 0 else 4)
        if STOP == "pre":
            return
        for f in range(KF):
            slot = load_unit(wgu_d[l, j, f], wgu_b[l, j, f], 2048, first)
            wv = slot[:, 0:2048].rearrange("p (g c n) -> p g c n", g=2, c=KD)
            for tc in range(NTC):
                sl = slice(tc * 512, (tc + 1) * 512)
                G = bank()
                Ub_ = bank()
                for k in range(KD):
                    mm(G, wv[:, 0, k, :], xn[:, k, sl], k == 0, k == KD - 1)
                for k in range(KD):
                    mm(Ub_, wv[:, 1, k, :], xn[:, k, sl], k == 0, k == KD - 1)
                sg = r_sg()
                act(sg, G, AF.Silu)
                tt("dve", hT[:, f, sl], sg, Ub_, ALU.mult)
        if STOP == "gu":
            return
        for dc in range(KD):
            slot = load_unit(wd_d[l, j, dc], wd_b[l, j, dc], KF * 128, first)
            wv = slot[:, 0:KF * 128].rearrange("p (f n) -> p f n", f=KF)
            for tc in range(NTC):
                sl = slice(tc * 512, (tc + 1) * 512)
                Y = bank()
                KX = os.environ.get("K_X", "")
                if KX == "loadonly":
                    continue
                for f in range(KF if KX != "mm1" else 1):
                    mm(Y, wv[:, f, :], hT[:, f, sl], f == 0, f == (KF - 1 if KX != "mm1" else 0))
                if KX == "noevac":
                    continue
                cp("dve", hb[:, dc, sl], Y)
                act(xn[:, dc, sl], hb[:, dc, sl], AF.Square)
        if STOP == "down":
            return
        for tc in range(NTC):
            postnorm(l, 1 if j == 0 else 5, tc, True)

    mo = {"o": 0}

    def carve(nwords, shape, dt=F32, parts=128):
        o = mo["o"]
        mo["o"] += nwords
        assert mo["o"] <= 11264, mo["o"]
        if dt == F32:
            v = U[0:parts, o:o + nwords]
        else:
            v = Ub[0:parts, 2 * o:2 * o + 2 * nwords]
        return v, shape

    def view(v_shape, pattern=None, **kw):
        v, shape = v_shape
        if pattern is None:
            return v
        return v.rearrange(pattern, **kw)

    gqT = view(carve(512, None))
    gkT = view(carve(512, None))
    glrT = view(carve(512, None, parts=16))
    uext = view(carve(542, None, BF16), "p (c t) -> p c t", c=2)
    cacc = view(carve(1024, None), "p (c t) -> p c t", c=2)
    sig = view(carve(512, None))
    sqT = view(carve(1024, None, BF16), "p (c t) -> p c t", c=4)
    skT = view(carve(640, None, BF16), "p (v t) -> p v t", v=2)
    gkt = view(carve(512, None), "p (s d) -> p s d", s=4)
    gv = view(carve(512, None, BF16), "p (s d) -> p s d", s=4)
    grs = view(carve(1024, None), "p (s d) -> p s d", s=4)
    sv = view(carve(320, None, BF16), "p (s d) -> p s d", s=5)
    mixg = view(carve(512, None, BF16), "p (c t) -> p c t", c=2)
    mixc = view(carve(512, None, BF16), "p (c t) -> p c t", c=2)
    mixs = view(carve(2048, None, BF16, parts=64), "p (c t) -> p c t", c=8)
    csq = sig.bitcast(BF16).rearrange("p (c t) -> p c t", c=2)

    r_lp = ring("lp", 2, [128, 128])
    r_eb = ring("eb", 2, [128, 128])
    r_enb = ring("enb", 2, [128, 128])
    r_ec = ring("ec", 2, [128, 128])
    r_kt = ring("kt", 2, [128, 128], BF16)
    r_kh = ring("kh", 2, [128, 128], BF16)
    r_atm = ring("atm", 2, [128, 512], BF16)
    r_osb = ring("osb", 2, [128, 256])
    r_osq = ring("osq", 1, [128, 256])
    r_st4 = ring("st4", 4, [128, 4])
    r_og = ring("og", 2, [128, 256], BF16)
    r_s = ring("s", 2, [128, 2, 256])
    r_pb = ring("pbf", 2, [128, 4, 256], BF16)
    r_pT = ring("pT", 2, [128, 2, 512], BF16)
    r_st2 = ring("st2", 8, [128, 2])
    r_mu = ring("mu", 1, [128, 512])
    r_t1 = r_sg

    def mixer_half(l, t, half, first):
        tok0 = half * 512
        ppl = pp[l]
        very_first = (t == 0 and half == 0)
        cp("pool", uext[:, :, 0:30], utail[l])
        cp("pool", skT[:, :, 0:128], kprev[l])
        cp("pool", sv[:, 0, :], vprev[l])

        def fm_chunk(wv, gi, ncols=128):
            b = bank()
            for k in range(KD):
                mm(b[0:ncols, :], wv[:, gi, k, 0:ncols], xn[:, k, tok0:tok0 + 512], k == 0, k == KD - 1)
            return b

        units = {}

        def unit(un):
            slot = load_unit(win_d[l, un], win_b[l, un], 2048, first)
            return slot

        u0 = unit(0)[:, 0:2048].rearrange("p (g c n) -> p g c n", g=2, c=KD)
        b = fm_chunk(u0, 0)
        cp("act", gqT, b)
        b = fm_chunk(u0, 1)
        cp("act", gkT, b)
        u6 = unit(6)[:, 0:2048].rearrange("p (g c n) -> p g c n", g=2, c=KD)
        b = fm_chunk(u6, 0, 16)
        cp("act", glrT, b[0:16, :])
        u2 = unit(2)[:, 0:2048].rearrange("p (g c n) -> p g c n", g=2, c=KD)
        u1 = unit(1)[:, 0:2048].rearrange("p (g c n) -> p g c n", g=2, c=KD)
        for c in range(2):
            b = fm_chunk(u2, c)
            act(sig, b, AF.Sigmoid)
            b2 = fm_chunk(u1, c)
            tt("dve", uext[:, c, 30:542], b2, sig, ALU.mult)
        for un in (3, 4):
            uu = unit(un)[:, 0:2048].rearrange("p (g c n) -> p g c n", g=2, c=KD)
            for gi in range(2):
                b = fm_chunk(uu, gi)
                cp("act", sqT[:, (un - 3) * 2 + gi, :], b)
        u5 = unit(5)[:, 0:2048].rearrange("p (g c n) -> p g c n", g=2, c=KD)
        for kv in range(2):
            b = fm_chunk(u5, kv)
            cp("dve", skT[:, kv, 128:640], b)
        tmu = [unit(7 + i)[:, 0:2048].rearrange("p (c n) -> p c n", c=KD) for i in range(3)]
        for s in range(4):
            ts_ = slice(tok0 + s * 128, tok0 + (s + 1) * 128)
            bs = []
            for i in range(3):
                b = bank()
                for k in range(KD):
                    mm(b[:, 0:256], xn[:, k, ts_], tmu[i][:, k, :], k == 0, k == KD - 1)
                bs.append(b)
            cp("dve", gkt[:, s, :], bs[0][:, 0:128])
            cp("act", sv[:, 1 + s, :], bs[0][:, 128:256])
            cp("act", gv[:, s, :], bs[1][:, 0:256])
            act(grs[:, s, :], bs[2][:, 0:256], AF.Silu)
        for s in range(4):
            tt("pool", grs[:, s, :], grs[:, s, :], pb[l][:, 0:256], ALU.mult)

        MS = os.environ.get("K_MSTOP", "")
        if MS == "proj":
            return
        for c in range(2):
            cb_ = bank()
            for w in range(31):
                mm(cb_, cdiag[l][:, c * 31 + w, :], uext[:, c, w:w + 512], w == 0, w == 30)
            act(cacc[:, c, :], cb_, AF.Identity, bias=ppl[:, PP_CB + c:PP_CB + c + 1])
            cp("pool", utail[l][:, c, :], uext[:, c, 512:542])

        if MS == "conv":
            return
        for s in range(4):
            ssl = slice(s * 128, (s + 1) * 128)
            zb = bank()
            mm(zb[:, 0:128], glrT[0:16, ssl], w2[l], True, False)
            mm(zb[:, 0:128], ones_f[0:1, 0:128], bg[l], False, True)
            lp = r_lp()
            act(lp, zb[:, 0:128], AF.Exp, scale=-1.0)
            act(lp, lp, AF.Ln, bias=1.0)
            bb = bank()
            mm(bb[:, 0:128], lp, trin_f, True, True)
            mm(bb[:, 128:256], tric_f, lp, True, True)
            eb = r_eb()
            act(eb, bb[:, 0:128], AF.Exp)
            enb = r_enb()
            act(enb, bb[:, 0:128], AF.Exp, scale=-1.0)
            ec = r_ec()
            act(ec, bb[:, 128:256], AF.Exp)
            for hh in range(4):
                ps = slice(32 * hh, 32 * hh + 32)
                stt("dve", qblk[ps, hh, :], gqT[ps, ssl], 32.0 ** -0.5, eb[ps, :], ALU.mult, ALU.mult)
            kt = r_kt()
            tt("pool", kt, gkT[:, ssl], enb, ALU.mult)
            kh = r_kh()
            tt("pool", kh, gkt[:, s, :], ec, ALU.mult)
            ab = bank()
            mm(ab, kt, qblk.rearrange("p h i -> p (h i)"), True, True)
            atm = r_atm()
            tt("dve", atm.rearrange("p (h i) -> p h i", h=4), ab.rearrange("p (h i) -> p h i", h=4),
               mask_b.unsqueeze(1).broadcast_to([128, 4, 128]), ALU.mult,
               rd=[ab, mask_b], wr=[atm])
            ob = bank()
            for hh in range(4):
                mm(ob[:, hh * 64:(hh + 1) * 64], atm[:, hh * 128:(hh + 1) * 128], gv[:, s, hh * 64:(hh + 1) * 64],
                   True, False)
                mm(ob[:, hh * 64:(hh + 1) * 64], qblk[:, hh, :], Sbf[l][:, hh * 64:(hh + 1) * 64], False, True)
            ub = bank()
            mm(ub[:, 0:256], kh, gv[:, s, :], True, True)
            stt("dve", S32[l], S32[l], eb[:, 127:128], ub[:, 0:256], ALU.mult, ALU.add)
            cp("pool", Sbf[l], S32[l])
            osb = r_osb()
            cp("act", osb, ob[:, 0:256])
            osq = r_osq()
            tt("pool", osq, osb, osb, ALU.mult)
            ssq = r_st4()
            P.op("dve", lambda h, ssq=ssq, osq=osq: h.reduce_sum(
                out=ssq, in_=osq.rearrange("p (h d) -> p h d", h=4), axis=AX.X),
                reads=[osq], writes=[ssq])
            lnv = r_st4()
            act(lnv, ssq, AF.Ln, bias=1e-6, scale=1.0 / 64.0)
            rn = r_st4()
            act(rn, lnv, AF.Exp, scale=-0.5)
            tt("pool", osb.rearrange("p (h d) -> p h d", h=4), osb.rearrange("p (h d) -> p h d", h=4),
               rn.unsqueeze(2).broadcast_to([128, 4, 64]), ALU.mult, rd=[osb, rn], wr=[osb])
            og = r_og()
            tt("pool", og, osb, grs[:, s, :], ALU.mult)
            tb = bank().bitcast(BF16)
            for c in range(2):
                tr(tb[:, c * 128:(c + 1) * 128], og[:, c * 128:(c + 1) * 128], ident_b)
            cp("act", mixg[:, :, ssl], tb[:, 0:256].rearrange("p (c t) -> p c t", c=2),
               rd=[tb], wr=[mixg[:, 0, ssl], mixg[:, 1, ssl]])

        if MS == "gla":
            return
        for n in range(4):
            qsl = slice(n * 128, (n + 1) * 128)
            for kv in range(2):
                pbf = r_pb()
                for pr in range(2):
                    h0 = kv * 4 + pr * 2
                    sbk = bank()
                    for gi in range(2):
                        hh = kv * 4 + pr + 2 * gi
                        po = (hh % 2) * 64
                        mm(sbk[:, gi * 256:(gi + 1) * 256], sqT[po:po + 64, hh // 2, qsl],
                           skT[po:po + 64, kv, n * 128:n * 128 + 256], True, True)
                    s_ = r_s()
                    stt("dve", s_, sbk.rearrange("p (g k) -> p g k", g=2), 0.125, swab[:, h0:h0 + 2, :],
                        ALU.mult, ALU.add, rd=[sbk, swab[:, h0, :], swab[:, h0 + 1, :]], wr=[s_])
                    if very_first and n == 0:
                        memset("dve", s_[:, :, 0:128], NEG, wr=[s_])
                    m = r_st2()
                    P.op("dve", lambda h, m=m, s_=s_: h.reduce_max(out=m, in_=s_, axis=AX.X),
                         reads=[s_], writes=[m])
                    negm = r_st2()
                    stt("dve", negm, m, -1.0, nsink[l][:, h0:h0 + 2], ALU.mult, ALU.min)
                    dsk = r_st2()
                    tt("dve", dsk, pb[l][:, 256 + h0:256 + h0 + 2], negm, ALU.add)
                    esk = r_st2()
                    act(esk, dsk, AF.Exp)
                    p_ = s_
                    rsum = r_st2()
                    for gi in range(2):
                        act(p_[:, gi, :], s_[:, gi, :], AF.Exp, bias=negm[:, gi:gi + 1], accum=rsum[:, gi:gi + 1],
                            rd=[s_], wr=[p_])
                    den = r_st2()
                    tt("dve", den, rsum, esk, ALU.add)
                    rden = r_st2()
                    recip(rden, den)
                    for gi in range(2):
                        act(pbf[:, pr + 2 * gi, :], p_[:, gi, :], AF.Identity, scale=rden[:, gi:gi + 1],
                            rd=[p_], wr=[pbf])
                tb = bank().bitcast(BF16)
                for kb in range(2):
                    for g in range(4):
                        tr(tb[:, kb * 512 + g * 128:kb * 512 + (g + 1) * 128],
                           pbf[:, g, kb * 128:(kb + 1) * 128], ident_b)
                pT = r_pT()
                cp("act", pT.rearrange("p k q -> p (k q)"), tb, rd=[tb], wr=[pT])
                ob = bank()
                for kb in range(2):
                    mm(ob[0:64, :], sv[:, n + kb, kv * 64:(kv + 1) * 64], pT[:, kb, :], kb == 0, kb == 1)
                cp("act", mixs[:, kv * 4:(kv + 1) * 4, qsl], ob[0:64, :].rearrange("p (g q) -> p g q", g=4),
                   rd=[ob], wr=[mixs[:, kv * 4 + g, qsl] for g in range(4)])
        cp("pool", kprev[l], skT[:, :, 512:640])
        cp("pool", vprev[l], sv[:, 4, :])

        if MS == "swa":
            return
        for c in range(2):
            act(csq[:, c, :], cacc[:, c, :], AF.Square)
        mb = bank()
        for c in range(2):
            mm(mb, ones_f, cacc[:, c, :], c == 0, c == 1)
        qb = bank()
        for c in range(2):
            mm(qb, ones_b, csq[:, c, :], c == 0, c == 1)
        mu = r_mu()
        act(mu, mb, AF.Copy, scale=1.0 / 256.0)
        musq = r_t1()
        tt("pool", musq, mu, mu, ALU.mult)
        var = r_t1()
        stt("dve", var, qb, 1.0 / 256.0, musq, ALU.mult, ALU.subtract)
        rs = r_sd()
        act(rs, var, AF.Sqrt, bias=1e-5)
        recip(rs, rs)
        for c in range(2):
            t1 = r_t1()
            tt("dve", t1, cacc[:, c, :], mu, ALU.subtract)
            tt("dve", t1, t1, rs, ALU.mult)
            act(mixc[:, c, :], t1, AF.Silu, bias=ppl[:, PP_LB + c:PP_LB + c + 1], scale=ppl[:, PP_LG + c:PP_LG + c + 1])

        if MS == "ln":
            return
        for dc in range(KD):
            slot = load_unit(wo_d[l, dc], wo_b[l, dc], 1536, first)
            wv = slot[:, 0:1536].rearrange("p (k n) -> p k n", k=12)
            Y = bank()
            for kc in range(12):
                if kc < 2:
                    mm(Y, wv[:, kc, :], mixg[:, kc, :], kc == 0, False)
                elif kc < 4:
                    mm(Y, wv[:, kc, :], mixc[:, kc - 2, :], False, False)
                else:
                    mm(Y, wv[0:64, kc, :], mixs[:, kc - 4, :], False, kc == 11)
            sl = slice(tok0, tok0 + 512)
            cp("dve", hb[:, dc, sl], Y)
            act(xn[:, dc, sl], hb[:, dc, sl], AF.Square)
        postnorm(l, 3, half, False)

    for t in range(NT):
        first = (t == 0)
        for c in range(KD):
            dma(X[:, c, :], xT[c * 128:(c + 1) * 128, t * T:(t + 1) * T], "xin")
        for l in range(L):
            if dbg == ("init", l):
                break
            build_cdiag(l)
            ffn(l, 0, first)
            if dbg == ("ffn1", l):
                break
            prenorm(l, 2)
            for half in range(NTC):
                mixer_half(l, t, half, first)
            if dbg == ("mix", l):
                break
            ffn(l, 1, first)
        if first:
            flush_stores()
        for c in range(KD):
            dma(outT[c * 128:(c + 1) * 128, t * T:(t + 1) * T], X[:, c, :], "xout")

    with nc.allow_low_precision("bf16 matmuls with fp32 accumulation"):
        P.emit(final_wait_chans=["xout"])
    if os.environ.get("K_VERBOSE"):
        print("sbuf bytes remaining", nc.sbuf_bytes_remaining, {e: len(v) for e, v in P.ops.items()})
    return nc, P


_CACHE = {}


def _run(inp, S, L, n_cores, dbg=None, trace=False):
    key = (S, L, dbg)
    if key not in _CACHE:
        _CACHE[key] = build(S, L, dbg=dbg)
    nc, _ = _CACHE[key]
    wts = _prep_weights(inp, L)
    cst = _const_table()
    x = np.asarray(inp["x"], np.float32)
    in_maps = []
    for b in range(n_cores):
        m = dict(wts)
        m["cst"] = cst
        m["xT"] = np.ascontiguousarray(x[b].T)
        in_maps.append(m)
    res = run_bass_kernel_spmd(nc, in_maps, core_ids=list(range(n_cores)), trace=trace)
    out = np.stack([np.ascontiguousarray(r["outT"].T) for r in res.results], 0)
    return out, res


def kernel(x, norm_g, ffn_w_gate, ffn_w_up, ffn_w_down, w_in, gla_w_gate2, gla_b_gate,
           gla_norm_g, conv_w, conv_b, conv_ln_g, conv_ln_b, swa_sinks, w_out):
    inp = dict(x=x, norm_g=norm_g, ffn_w_gate=ffn_w_gate, ffn_w_up=ffn_w_up, ffn_w_down=ffn_w_down,
               w_in=w_in, gla_w_gate2=gla_w_gate2, gla_b_gate=gla_b_gate, gla_norm_g=gla_norm_g,
               conv_w=conv_w, conv_b=conv_b, conv_ln_g=conv_ln_g, conv_ln_b=conv_ln_b,
               swa_sinks=swa_sinks, w_out=w_out)
    inp = {k: np.asarray(v, np.float32) for k, v in inp.items()}
    B, S, _ = inp["x"].shape
    L = inp["norm_g"].shape[0]
    out, _ = _run(inp, S, L, B)
    return out.astype(np.float32)
```

```python
import contextlib
import numpy as np
import concourse.bass as bass
import concourse.mybir as mybir
from concourse.bass_utils import run_bass_kernel_spmd

F32 = mybir.dt.float32
BF16 = mybir.dt.bfloat16
AF = mybir.ActivationFunctionType
ALU = mybir.AluOpType
AX = mybir.AxisListType

_DSZ = {F32: 4, BF16: 2}

D = 1024
KD = 8
FF = 2816
KF = 22
NEG = -30000.0


def _region(ap):
    t = ap.tensor
    kind = type(t).__name__
    dims = ap.ap
    off = int(ap.offset)
    esz = _DSZ[ap.dtype]
    if kind.startswith("DRam"):
        ext = sum((c - 1) * abs(s) for s, c in dims) + 1
        return (t.name, 0, 1, off * esz, (off + ext) * esz)
    pstep, npart = dims[0]
    if pstep <= 0:
        p_lo, p_hi, fo = 0, 128, off
    else:
        p_lo = off // pstep
        p_hi = p_lo + npart
        fo = off % pstep
    if kind.startswith("PSum"):
        return (t.name, 0, 128, 0, 1 << 30)
    ext = sum((c - 1) * abs(s) for s, c in dims[1:]) + 1
    return (t.name, p_lo, p_hi, fo * esz, (fo + ext) * esz)


def _overlap(a, b):
    return a[1] < b[2] and b[1] < a[2] and a[3] < b[4] and b[3] < a[4]


def _covers(a, b):
    return a[1] <= b[1] and a[2] >= b[2] and a[3] <= b[3] and a[4] >= b[4]


class Prog:
    ENGS = ("pe", "act", "dve", "pool", "sp")

    def __init__(self, nc):
        self.nc = nc
        self.ops = {e: [] for e in self.ENGS}
        self.wrec = {}
        self.rrec = {}
        self.dma_cnt = {}
        self.same_eng_sync = {"act", "dve", "pool"}

    def op(self, eng, fn, reads=(), writes=(), chan=None, group=False):
        deps_e = {}
        deps_d = {}

        def add(tok):
            if tok[0] == "e":
                _, e, i = tok
                if e == eng and e not in self.same_eng_sync:
                    return
                if deps_e.get(e, -1) < i:
                    deps_e[e] = i
            else:
                _, c, n = tok
                if deps_d.get(c, 0) < n:
                    deps_d[c] = n

        rregs = [_region(a) for a in reads]
        wregs = [_region(a) for a in writes]
        for r in rregs:
            for (reg, tok) in self.wrec.get(r[0], ()):
                if _overlap(reg, r):
                    add(tok)
            if r[0].startswith("bank"):
                for (reg, tok) in self.rrec.get(r[0], ()):
                    if tok[0] == "e" and tok[1] != eng:
                        add(tok)
        for w in wregs:
            for (reg, tok) in self.wrec.get(w[0], ()):
                if _overlap(reg, w):
                    add(tok)
            for (reg, tok) in self.rrec.get(w[0], ()):
                if _overlap(reg, w):
                    add(tok)
        idx = len(self.ops[eng])
        if chan is None:
            tok = ("e", eng, idx)
        else:
            n = self.dma_cnt.get(chan, 0) + 1
            self.dma_cnt[chan] = n
            tok = ("d", chan, (1 << 30) if group else n)
        for r in rregs:
            lst = self.rrec.setdefault(r[0], [])
            if tok[0] == "e":
                lst[:] = [(g, t) for (g, t) in lst
                          if not (t[0] == "e" and t[1] == eng and _covers(r, g))]
            lst.append((r, tok))
        for w in wregs:
            wl = self.wrec.setdefault(w[0], [])
            wl[:] = [(g, t) for (g, t) in wl if not _covers(w, g)]
            wl.append((w, tok))
            rl = self.rrec.get(w[0])
            if rl:
                rl[:] = [(g, t) for (g, t) in rl if not _covers(w, g)]
        self.ops[eng].append(dict(fn=fn, de=deps_e, dd=deps_d, chan=chan, sig=False))
        return tok

    def emit(self, final_wait_chans=()):
        nc = self.nc
        ops = self.ops
        for e in self.ENGS:
            for o in ops[e]:
                for (f, i) in o["de"].items():
                    ops[f][i]["sig"] = True
        cum = {}
        for e in self.ENGS:
            c = 0
            arr = []
            for o in ops[e]:
                if o["sig"]:
                    c += 1
                arr.append(c)
            cum[e] = arr
        chans = sorted(self.dma_cnt.keys())
        with contextlib.ExitStack() as st:
            esem = {e: st.enter_context(nc.semaphore("s_" + e)) for e in self.ENGS}
            dsem = {c: st.enter_context(nc.semaphore("d_" + str(c))) for c in chans}
            block = st.enter_context(nc.Block())

            def run(e, h):
                waited_e = {}
                waited_d = {}
                for o in ops[e]:
                    for (f, i) in o["de"].items():
                        tgt = cum[f][i]
                        if waited_e.get(f, 0) < tgt:
                            h.wait_ge(esem[f], tgt)
                            waited_e[f] = tgt
                    for (c, n) in o["dd"].items():
                        n = min(n, self.dma_cnt[c])
                        if waited_d.get(c, 0) < n:
                            h.wait_ge(dsem[c], 16 * n)
                            waited_d[c] = n
                    ins = o["fn"](h)
                    if o["chan"] is not None:
                        ins.then_inc(dsem[o["chan"]], 16)
                    if o["sig"]:
                        ins.then_inc(esem[e], 1)
                if e == "sp":
                    for c in final_wait_chans:
                        h.wait_ge(dsem[c], 16 * self.dma_cnt[c])

            @block.tensor
            def _(h):
                run("pe", h)

            @block.scalar
            def _(h):
                run("act", h)

            @block.vector
            def _(h):
                run("dve", h)

            @block.gpsimd
            def _(h):
                run("pool", h)

            @block.sync
            def _(h):
                run("sp", h)


C_ID, C_ONE, C_TRIN, C_TRIC, C_MASK, C_BIAS = 0, 128, 256, 384, 512, 640
NCONST = 640 + 8 * 256


_HPERM = [0, 2, 1, 3, 4, 6, 5, 7]


def _const_table():
    c = np.zeros((128, NCONST), np.float32)
    j = np.arange(128)[:, None]
    i = np.arange(128)[None, :]
    c[:, C_ID:C_ID + 128] = (j == i)
    c[:, C_ONE:C_ONE + 128] = 1.0
    c[:, C_TRIN:C_TRIN + 128] = np.where(j <= i, -1.0 / 16.0, 0.0)
    c[:, C_TRIC:C_TRIC + 128] = np.where(j > i, -1.0 / 16.0, 0.0)
    c[:, C_MASK:C_MASK + 128] = (j <= i)
    q = np.arange(128)[:, None]
    kj = np.arange(256)[None, :]
    dist = q - kj + 128
    valid = (dist >= 0) & (dist < 128)
    for jpos, h in enumerate(_HPERM):
        slope = 2.0 ** (-(h + 1))
        c[:, C_BIAS + jpos * 256:C_BIAS + (jpos + 1) * 256] = np.where(valid, -slope * dist, NEG)
    return c


_O_GQ, _O_GK, _O_GV, _O_GR, _O_GLR, _O_CA, _O_CG, _O_SQ, _O_SK, _O_SV = (
    0, 128, 256, 512, 768, 784, 1040, 1296, 1808, 1936)

PP_G, PP_CW, PP_CB, PP_LG, PP_LB, NPP = 0, 48, 110, 112, 114, 116


def _fm(v, nch):
    return np.ascontiguousarray(v.reshape(nch, 128).T)


def _prep_weights(inp, L):
    f32 = np.float32
    wgu = np.empty((L, 2, KF, 128, 2048), f32)
    wd = np.empty((L, 2, KD, 128, KF * 128), f32)
    win = np.zeros((L, 10, 128, 2048), f32)
    wo = np.zeros((L, KD, 128, 12 * 128), f32)
    pp = np.zeros((L, 128, NPP), f32)
    pb = np.zeros((L, 264), f32)
    w2 = np.ascontiguousarray(inp["gla_w_gate2"][:L]).astype(f32)
    bg = np.ascontiguousarray(inp["gla_b_gate"][:L]).astype(f32).reshape(L, 1, 128)
    for l in range(L):
        for j in range(2):
            g = inp["ffn_w_gate"][l, j].reshape(KD, 128, KF, 128)
            u = inp["ffn_w_up"][l, j].reshape(KD, 128, KF, 128)
            wgu[l, j] = np.stack([g, u], 0).transpose(3, 2, 0, 1, 4).reshape(KF, 128, 2048)
            dn = inp["ffn_w_down"][l, j].reshape(KF, 128, KD, 128)
            wd[l, j] = dn.transpose(2, 1, 0, 3).reshape(KD, 128, KF * 128)
        w = inp["w_in"][l]
        cols = [w[:, _O_GQ:_O_GQ + 128], w[:, _O_GK:_O_GK + 128]]
        cols += [w[:, _O_CA + 128 * i:_O_CA + 128 * (i + 1)] for i in range(2)]
        cols += [w[:, _O_CG + 128 * i:_O_CG + 128 * (i + 1)] for i in range(2)]
        cols += [w[:, _O_SQ + 128 * i:_O_SQ + 128 * (i + 1)] for i in range(4)]
        for kv in range(2):
            k = w[:, _O_SK + 64 * kv:_O_SK + 64 * (kv + 1)]
            cols.append(np.concatenate([k, k], 1))
        glr = np.zeros((D, 128), f32)
        glr[:, :16] = w[:, _O_GLR:_O_GLR + 16]
        cols.append(glr)
        cols.append(np.zeros((D, 128), f32))
        for un in range(7):
            a = cols[2 * un].reshape(KD, 128, 128)
            b = cols[2 * un + 1].reshape(KD, 128, 128)
            win[l, un] = np.stack([a, b], 0).transpose(2, 0, 1, 3).reshape(128, 2048)
        tm = [np.concatenate([w[:, _O_GK:_O_GK + 128], w[:, _O_SV:_O_SV + 128]], 1),
              w[:, _O_GV:_O_GV + 256], w[:, _O_GR:_O_GR + 256]]
        for un in range(3):
            win[l, 7 + un] = tm[un].reshape(KD, 128, 256).transpose(1, 0, 2).reshape(128, 2048)
        o = inp["w_out"][l]
        for dc in range(KD):
            blk = np.zeros((128, 12, 128), f32)
            oc = o[:, dc * 128:(dc + 1) * 128]
            blk[:, 0:4, :] = oc[0:512].reshape(4, 128, 128).transpose(1, 0, 2)
            blk[0:64, 4:12, :] = oc[512:1024].reshape(8, 64, 128).transpose(1, 0, 2)
            wo[l, dc] = blk.reshape(128, 1536)
        pp[l, :, PP_G:PP_G + 48] = inp["norm_g"][l].reshape(6, KD, 128).transpose(2, 0, 1).reshape(128, 48)
        pp[l, :, PP_CW:PP_CW + 62] = inp["conv_w"][l].reshape(31, 2, 128).transpose(2, 1, 0).reshape(128, 62)
        pp[l, :, PP_CB:PP_CB + 2] = _fm(inp["conv_b"][l], 2)
        pp[l, :, PP_LG:PP_LG + 2] = _fm(inp["conv_ln_g"][l], 2)
        pp[l, :, PP_LB:PP_LB + 2] = _fm(inp["conv_ln_b"][l], 2)
        pb[l, 0:256] = inp["gla_norm_g"][l].reshape(256)
        pb[l, 256:264] = inp["swa_sinks"][l][_HPERM]
    return dict(wgu=wgu, wd=wd, win=win, wo=wo, pp=pp, pb=pb.reshape(L, 1, 264), w2=w2, bg=bg)


import os
POOL = os.environ.get("K_POOL", "pool")


def build(S, L, T=512, dbg=None):
    assert S % T == 0 and T % 512 == 0
    NT = S // T
    NTC = T // 512
    nc = bass.Bass("TRN2", target_bir_lowering=False)
    P = Prog(nc)

    def dram(name, shape, dt=F32, kind="ExternalInput"):
        return nc.dram_tensor(name, list(shape), dt, kind=kind).ap()

    xT = dram("xT", [D, S])
    outT = dram("outT", [D, S], kind="ExternalOutput")
    cst_d = dram("cst", [128, NCONST])
    pp_d = dram("pp", [L, 128, NPP])
    pb_d = dram("pb", [L, 1, 264])
    w2_d = dram("w2", [L, 16, 128])
    bg_d = dram("bg", [L, 1, 128])
    wgu_d = dram("wgu", [L, 2, KF, 128, 2048])
    wd_d = dram("wd", [L, 2, KD, 128, KF * 128])
    win_d = dram("win", [L, 10, 128, 2048])
    wo_d = dram("wo", [L, KD, 128, 1536])
    wgu_b = dram("wgu_b", [L, 2, KF, 128, 2048], BF16, kind="Internal")
    wd_b = dram("wd_b", [L, 2, KD, 128, KF * 128], BF16, kind="Internal")
    win_b = dram("win_b", [L, 10, 128, 2048], BF16, kind="Internal")
    wo_b = dram("wo_b", [L, KD, 128, 1536], BF16, kind="Internal")

    def sb(name, shape, dt=F32):
        return nc.alloc_sbuf_tensor(name, list(shape), dt).ap()

    X = sb("X", [128, KD, T])
    xn = sb("xn", [128, KD, T], BF16)
    U = sb("U", [128, 15360])
    Ub = U.bitcast(BF16)
    hT = Ub[:, 0:KF * T].rearrange("p (f t) -> p f t", f=KF)
    hb = U[:, 11264:11264 + KD * T].rearrange("p (c t) -> p c t", c=KD)
    sqpre = Ub[:, 0:KD * T].rearrange("p (c t) -> p c t", c=KD)

    NW = 4
    wslots = [sb(f"wsl{i}", [128, 3072], BF16) for i in range(NW)]
    NST = 2
    stages = [sb(f"stg{i}", [128, 2048]) for i in range(NST)]
    cst = sb("cstf", [128, NCONST])
    cbf = sb("cstb", [128, 640], BF16)
    ident_b = cbf[:, C_ID:C_ID + 128]
    ones_b = cbf[:, C_ONE:C_ONE + 128]
    mask_b = cbf[:, C_MASK:C_MASK + 128]
    ones_f = cst[:, C_ONE:C_ONE + 128]
    trin_f = cst[:, C_TRIN:C_TRIN + 128]
    tric_f = cst[:, C_TRIC:C_TRIC + 128]
    swab = cst[:, C_BIAS:C_BIAS + 2048].rearrange("p (h k) -> p h k", h=8)

    pp = [sb(f"pp{l}", [128, NPP]) for l in range(L)]
    g05 = [sb(f"g05_{l}", [128, 48]) for l in range(L)]
    pb = [sb(f"pb{l}", [128, 264]) for l in range(L)]
    w2 = [sb(f"w2_{l}", [16, 128]) for l in range(L)]
    bg = [sb(f"bg_{l}", [1, 128]) for l in range(L)]
    nsink = [sb(f"nsink{l}", [128, 8]) for l in range(L)]
    S32 = [sb(f"S32_{l}", [128, 256]) for l in range(L)]
    Sbf = [sb(f"Sbf_{l}", [128, 256], BF16) for l in range(L)]
    utail = [sb(f"utail{l}", [128, 2, 30], BF16) for l in range(L)]
    kprev = [sb(f"kprev{l}", [128, 2, 128], BF16) for l in range(L)]
    vprev = [sb(f"vprev{l}", [128, 128], BF16) for l in range(L)]

    def ring(name, n, shape, dt=F32):
        lst = [sb(f"{name}{i}", shape, dt) for i in range(n)]
        st = {"i": 0}

        def nxt():
            a = lst[st["i"] % n]
            st["i"] += 1
            return a
        return nxt

    r_sg = ring("sg", 2, [128, 512])
    r_sd = ring("sd", 2, [128, 512])

    banks = [nc.alloc_psum_tensor(f"bank{i}", [128, 512], F32).ap() for i in range(8)]
    bstate = {"i": 0}

    def bank():
        b = banks[bstate["i"] % 8]
        bstate["i"] += 1
        return b

    def mm(out, lhsT, rhs, start=True, stop=True):
        P.op("pe", lambda h: h.matmul(out, lhsT=lhsT, rhs=rhs, start=start, stop=stop),
             reads=[lhsT, rhs], writes=[out])

    def tr(out, in_, ident):
        P.op("pe", lambda h: h.transpose(out, in_, ident), reads=[in_, ident], writes=[out])

    def act(out, in_, func, bias=None, scale=None, accum=None, eng="act", rd=None, wr=None):
        kw = {}
        rds = [in_] if rd is None else list(rd)
        if bias is not None:
            kw["bias"] = bias
            if not isinstance(bias, (int, float)):
                rds.append(bias)
        if scale is not None:
            kw["scale"] = scale
            if not isinstance(scale, (int, float)):
                rds.append(scale)
        wrs = [out] if wr is None else list(wr)
        if accum is not None:
            kw["accum_out"] = accum
            wrs.append(accum)
        P.op("act", lambda h: h.activation(out=out, in_=in_, func=func, **kw), reads=rds, writes=wrs)

    def tt(eng, out, a, b, op, rd=None, wr=None):
        P.op(eng, lambda h: h.tensor_tensor(out=out, in0=a, in1=b, op=op),
             reads=[a, b] if rd is None else rd, writes=[out] if wr is None else wr)

    def ts(eng, out, a, s1, op0, s2=None, op1=None, rd=None, wr=None):
        rds = [a] if rd is None else list(rd)
        for s in (s1, s2):
            if s is not None and not isinstance(s, (int, float)):
                rds.append(s)
        if op1 is None:
            P.op(eng, lambda h: h.tensor_scalar(out=out, in0=a, scalar1=s1, scalar2=None, op0=op0),
                 reads=rds, writes=[out] if wr is None else wr)
        else:
            P.op(eng, lambda h: h.tensor_scalar(out=out, in0=a, scalar1=s1, scalar2=s2, op0=op0, op1=op1),
                 reads=rds, writes=[out] if wr is None else wr)

    def stt(eng, out, a, s, b, op0, op1, rd=None, wr=None):
        rds = [a, b] if rd is None else list(rd)
        if not isinstance(s, (int, float)):
            rds.append(s)
        P.op(eng, lambda h: h.scalar_tensor_tensor(out=out, in0=a, scalar=s, in1=b, op0=op0, op1=op1),
             reads=rds, writes=[out] if wr is None else wr)

    def cp(eng, out, in_, rd=None, wr=None):
        if eng == "act":
            P.op("act", lambda h: h.copy(out=out, in_=in_), reads=[in_] if rd is None else rd,
                 writes=[out] if wr is None else wr)
        else:
            P.op(eng, lambda h: h.tensor_copy(out=out, in_=in_), reads=[in_] if rd is None else rd,
                 writes=[out] if wr is None else wr)

    def recip(out, in_):
        P.op("dve", lambda h: h.reciprocal(out=out, in_=in_), reads=[in_], writes=[out])

    def memset(eng, ap, v, wr=None):
        P.op(eng, lambda h: h.memset(ap, v), writes=[ap] if wr is None else wr)

    def dma(out, in_, chan, rd=None, wr=None, group=False):
        P.op("sp", lambda h: h.dma_start(out=out, in_=in_), reads=[in_] if rd is None else rd,
             writes=[out] if wr is None else wr, chan=chan, group=group)

    dma(cst, cst_d, "par", group=True)
    cp("dve", cbf, cst[:, 0:640])
    for l in range(L):
        dma(pp[l], pp_d[l], "par", group=True)
        dma(pb[l], pb_d[l].partition_broadcast(128), "par", group=True)
        dma(w2[l], w2_d[l], "par", group=True)
        dma(bg[l], bg_d[l], "par", group=True)
        ts("dve", g05[l], pp[l][:, PP_G:PP_G + 48], 0.5, ALU.mult)
        ts("dve", nsink[l], pb[l][:, 256:264], -1.0, ALU.mult)
        memset("dve", S32[l], 0.0)
        memset("dve", Sbf[l], 0.0)
        memset("dve", utail[l], 0.0)
        memset("dve", kprev[l], 0.0)
        memset("dve", vprev[l], 0.0)

    cdiag1 = sb("cdiag", [128, 62, 128], BF16)
    cdiag = [cdiag1 for l in range(L)]

    def build_cdiag(l):
        for k in range(62):
            tt("pool", cdiag1[:, k, :], cst[:, C_ID:C_ID + 128],
               pp[l][:, PP_CW + k:PP_CW + k + 1].broadcast_to([128, 128]), ALU.mult,
               rd=[cst[:, C_ID:C_ID + 128], pp[l][:, PP_CW + k:PP_CW + k + 1]])

    qblks = [sb(f"qblk{i}", [128, 4, 128], BF16) for i in range(4)]
    for q_ in qblks:
        memset("dve", q_, 0.0)
    khs = [sb(f"khs{i}", [128, 128], BF16) for i in range(4)]
    atms = [sb(f"atms{i}", [128, 512], BF16) for i in range(4)]
    ebs = [sb(f"ebs{i}", [128, 128]) for i in range(4)]

    wst = {"slot": 0, "stage": 0, "cast": 0, "pending": []}

    def load_unit(d32, dbf, n, first):
        si = wst["slot"] % NW
        wst["slot"] += 1
        slot = wslots[si]
        if first:
            off = 0
            while off < n:
                m = min(2048, n - off)
                gi = wst["stage"] % NST
                wst["stage"] += 1
                stg = stages[gi]
                dma(stg[:, 0:m], d32[:, off:off + m], f"stg{gi}")
                ce = "act"
                wst["cast"] += 1
                cp(ce, slot[:, off:off + m], stg[:, 0:m])
                off += m
            pend = wst["pending"]
            wst["pending"] = [(slot, dbf, n, si)]
            for (s_, d_, n_, si_) in pend:
                dma(d_, s_[:, 0:n_], f"wst{si_}")
        else:
            dma(slot[:, 0:n], dbf, f"wld{si}")
        return slot

    class Units:
        def __init__(self, specs, first, PF=2):
            self.specs = specs
            self.first = first
            self._pf = PF
            self.loaded = []

        @property
        def PF(self):
            return self._pf if os.environ.get("K_PF") == "1" else 0

        @PF.setter
        def PF(self, v):
            self._pf = v

        def get(self, i):
            while len(self.loaded) < min(len(self.specs), i + 1 + self.PF):
                d32, dbf, n = self.specs[len(self.loaded)]
                self.loaded.append(load_unit(d32, dbf, n, self.first))
            return self.loaded[i]

    def flush_stores():
        for (s_, d_, n_, si_) in wst["pending"]:
            dma(d_, s_[:, 0:n_], f"wst{si_}")
        wst["pending"] = []

    def rstd_from_sq(sqv, tc, n_feat_chunks, inv_n, eps):
        b = bank()
        for c in range(n_feat_chunks):
            mm(b, ones_b, sqv[:, c, tc * 512:(tc + 1) * 512], c == 0, c == n_feat_chunks - 1)
        sd = r_sd()
        act(sd, b, AF.Ln, bias=eps, scale=inv_n)
        act(sd, sd, AF.Exp, scale=-0.5)
        return sd

    def prenorm(l, n):
        for tc in range(NTC):
            sl = slice(tc * 512, (tc + 1) * 512)
            for c in range(KD):
                act(sqpre[:, c, sl], X[:, c, sl], AF.Square)
            rs = rstd_from_sq(sqpre, tc, KD, 1.0 / D, 1e-6)
            for c in range(KD):
                stt("dve", xn[:, c, sl], X[:, c, sl], pp[l][:, PP_G + n * 8 + c:PP_G + n * 8 + c + 1], rs,
                    ALU.mult, ALU.mult)

    def postnorm(l, n, tc, half_coef):
        sl = slice(tc * 512, (tc + 1) * 512)
        rs = rstd_from_sq(xn, tc, KD, 1.0 / D, 1e-6)
        gsrc = g05[l] if half_coef else pp[l]
        for c in range(KD):
            stt("dve", hb[:, c, sl], hb[:, c, sl], gsrc[:, n * 8 + c:n * 8 + c + 1], rs, ALU.mult, ALU.mult)
            tt("dve", X[:, c, sl], X[:, c, sl], hb[:, c, sl], ALU.add)

    STOP = os.environ.get("K_STOP", "")

    def ffn(l, j, first):
        prenorm(l, 0 if j == 0 else 4)
        if STOP == "pre":
            return
        un = Units([(wgu_d[l, j, f], wgu_b[l, j, f], 2048) for f in range(KF)] +
                   [(wd_d[l, j, dc], wd_b[l, j, dc], KF * 128) for dc in range(KD)], first)
        for f in range(KF):
            slot = un.get(f)
            wv = slot[:, 0:2048].rearrange("p (g c n) -> p g c n", g=2, c=KD)
            for tc in range(NTC):
                sl = slice(tc * 512, (tc + 1) * 512)
                G = bank()
                Ub_ = bank()
                for k in range(KD):
                    mm(G, wv[:, 0, k, :], xn[:, k, sl], k == 0, k == KD - 1)
                for k in range(KD):
                    mm(Ub_, wv[:, 1, k, :], xn[:, k, sl], k == 0, k == KD - 1)
                sg = r_sg()
                act(sg, G, AF.Silu)
                tt("dve", hT[:, f, sl], sg, Ub_, ALU.mult)
        if STOP == "gu":
            return
        for dc in range(KD):
            slot = un.get(KF + dc)
            wv = slot[:, 0:KF * 128].rearrange("p (f n) -> p f n", f=KF)
            for tc in range(NTC):
                sl = slice(tc * 512, (tc + 1) * 512)
                Y = bank()
                KX = os.environ.get("K_X", "")
                if KX == "loadonly":
                    continue
                for f in range(KF if KX != "mm1" else 1):
                    mm(Y, wv[:, f, :], hT[:, f, sl], f == 0, f == (KF - 1 if KX != "mm1" else 0))
                if KX == "noevac":
                    continue
                cp("dve", hb[:, dc, sl], Y)
                act(xn[:, dc, sl], hb[:, dc, sl], AF.Square)
        if STOP == "down":
            return
        for tc in range(NTC):
            postnorm(l, 1 if j == 0 else 5, tc, True)

    mo = {"o": 0}

    def carve(nwords, shape, dt=F32, parts=128):
        o = mo["o"]
        mo["o"] += nwords
        assert mo["o"] <= 11264, mo["o"]
        if dt == F32:
            v = U[0:parts, o:o + nwords]
        else:
            v = Ub[0:parts, 2 * o:2 * o + 2 * nwords]
        return v, shape

    def view(v_shape, pattern=None, **kw):
        v, shape = v_shape
        if pattern is None:
            return v
        return v.rearrange(pattern, **kw)

    gqT = view(carve(512, None))
    gkT = view(carve(512, None))
    glrT = view(carve(512, None, parts=16))
    uext = view(carve(542, None, BF16), "p (c t) -> p c t", c=2)
    cacc = view(carve(1024, None), "p (c t) -> p c t", c=2)
    sig = view(carve(512, None))
    sqT = view(carve(1024, None, BF16), "p (c t) -> p c t", c=4)
    skT = view(carve(640, None, BF16), "p (v t) -> p v t", v=2)
    gkt = view(carve(512, None), "p (s d) -> p s d", s=4)
    gv = view(carve(512, None, BF16), "p (s d) -> p s d", s=4)
    grs = view(carve(1024, None), "p (s d) -> p s d", s=4)
    sv = view(carve(320, None, BF16), "p (s d) -> p s d", s=5)
    mixg = view(carve(512, None, BF16), "p (c t) -> p c t", c=2)
    mixc = view(carve(512, None, BF16), "p (c t) -> p c t", c=2)
    mixs = view(carve(2048, None, BF16, parts=64), "p (c t) -> p c t", c=8)
    csq = sig.bitcast(BF16).rearrange("p (c t) -> p c t", c=2)

    r_lp = ring("lp", 2, [128, 128])
    r_enb = ring("enb", 2, [128, 128])
    r_ec = ring("ec", 2, [128, 128])
    r_kt = ring("kt", 2, [128, 128], BF16)
    r_osb = ring("osb", 2, [128, 256])
    r_osq = ring("osq", 1, [128, 256])
    r_st4 = ring("st4", 6, [128, 4])
    r_og = ring("og", 2, [128, 256], BF16)
    r_s = ring("s", 4, [128, 2, 256])
    r_pb = ring("pbf", 2, [128, 4, 256], BF16)
    r_pT = ring("pT", 2, [128, 2, 512], BF16)
    r_st2 = ring("st2", 40, [128, 2])
    r_mu = ring("mu", 1, [128, 512])
    r_t1 = r_sg

    def mixer_half(l, t, half, first):
        tok0 = half * 512
        ppl = pp[l]
        very_first = (t == 0 and half == 0)
        cp("pool", uext[:, :, 0:30], utail[l])
        cp("pool", skT[:, :, 0:128], kprev[l])
        cp("pool", sv[:, 0, :], vprev[l])

        def fm_chunk(wv, gi, ncols=128):
            b = bank()
            for k in range(KD):
                mm(b[0:ncols, :], wv[:, gi, k, 0:ncols], xn[:, k, tok0:tok0 + 512], k == 0, k == KD - 1)
            return b

        units = {}

        _order = [0, 6, 2, 1, 3, 4, 5, 7, 8, 9]
        mun = Units([(win_d[l, u_], win_b[l, u_], 2048) for u_ in _order] +
                    [(wo_d[l, dc], wo_b[l, dc], 1536) for dc in range(KD)], first, PF=1)

        def unit(un):
            return mun.get(_order.index(un))

        u0 = unit(0)[:, 0:2048].rearrange("p (g c n) -> p g c n", g=2, c=KD)
        b = fm_chunk(u0, 0)
        cp("act", gqT, b)
        b = fm_chunk(u0, 1)
        cp("act", gkT, b)
        u6 = unit(6)[:, 0:2048].rearrange("p (g c n) -> p g c n", g=2, c=KD)
        b = fm_chunk(u6, 0, 16)
        cp("act", glrT, b[0:16, :])
        u2 = unit(2)[:, 0:2048].rearrange("p (g c n) -> p g c n", g=2, c=KD)
        u1 = unit(1)[:, 0:2048].rearrange("p (g c n) -> p g c n", g=2, c=KD)
        for c in range(2):
            b = fm_chunk(u2, c)
            act(sig, b, AF.Sigmoid)
            b2 = fm_chunk(u1, c)
            tt("dve", uext[:, c, 30:542], b2, sig, ALU.mult)
        for un in (3, 4):
            uu = unit(un)[:, 0:2048].rearrange("p (g c n) -> p g c n", g=2, c=KD)
            for gi in range(2):
                b = fm_chunk(uu, gi)
                cp("act", sqT[:, (un - 3) * 2 + gi, :], b)
        u5 = unit(5)[:, 0:2048].rearrange("p (g c n) -> p g c n", g=2, c=KD)
        for kv in range(2):
            b = fm_chunk(u5, kv)
            cp("dve", skT[:, kv, 128:640], b)
        mun.PF = 2
        tmu = [unit(7 + i)[:, 0:2048].rearrange("p (c n) -> p c n", c=KD) for i in range(3)]
        mun.PF = 0
        for s in range(4):
            ts_ = slice(tok0 + s * 128, tok0 + (s + 1) * 128)
            bs = []
            for i in range(3):
                b = bank()
                for k in range(KD):
                    mm(b[:, 0:256], xn[:, k, ts_], tmu[i][:, k, :], k == 0, k == KD - 1)
                bs.append(b)
            cp("dve", gkt[:, s, :], bs[0][:, 0:128])
            cp("act", sv[:, 1 + s, :], bs[0][:, 128:256])
            cp("act", gv[:, s, :], bs[1][:, 0:256])
            act(grs[:, s, :], bs[2][:, 0:256], AF.Silu)
        for s in range(4):
            tt("pool", grs[:, s, :], grs[:, s, :], pb[l][:, 0:256], ALU.mult)

        MS = os.environ.get("K_MSTOP", "")
        if MS == "proj":
            return
        def conv_ln_lane(bank):
            for c in range(2):
                cb_ = bank()
                for w in range(31):
                    mm(cb_, cdiag[l][:, c * 31 + w, :], uext[:, c, w:w + 512], w == 0, w == 30)
                yield
                act(cacc[:, c, :], cb_, AF.Identity, bias=ppl[:, PP_CB + c:PP_CB + c + 1])
                cp("pool", utail[l][:, c, :], uext[:, c, 512:542])
                yield
            for c in range(2):
                act(csq[:, c, :], cacc[:, c, :], AF.Square)
            yield
            mb = bank()
            for c in range(2):
                mm(mb, ones_f, cacc[:, c, :], c == 0, c == 1)
            qb = bank()
            for c in range(2):
                mm(qb, ones_b, csq[:, c, :], c == 0, c == 1)
            yield
            mu = r_mu()
            act(mu, mb, AF.Copy, scale=1.0 / 256.0)
            yield
            musq = r_t1()
            tt("pool", musq, mu, mu, ALU.mult)
            yield
            var = r_t1()
            stt("dve", var, qb, 1.0 / 256.0, musq, ALU.mult, ALU.subtract)
            yield
            rs = r_sd()
            act(rs, var, AF.Ln, bias=1e-5)
            yield
            act(rs, rs, AF.Exp, scale=-0.5)
            yield
            for c in range(2):
                t1 = r_t1()
                ln_t1.append(t1)
                tt("dve", t1, cacc[:, c, :], mu, ALU.subtract)
                yield
                tt("dve", t1, t1, rs, ALU.mult)
                yield

        ln_t1 = []

        def gla_A(s, bank):
            ssl = slice(s * 128, (s + 1) * 128)
            zb = bank()
            mm(zb[:, 0:128], glrT[0:16, ssl], w2[l], True, False)
            mm(zb[:, 0:128], ones_f[0:1, 0:128], bg[l], False, True)
            yield
            lp = r_lp()
            act(lp, zb[:, 0:128], AF.Exp, scale=-1.0)
            yield
            act(lp, lp, AF.Ln, bias=1.0)
            yield
            bb = bank()
            mm(bb[:, 0:128], lp, trin_f, True, True)
            mm(bb[:, 128:256], tric_f, lp, True, True)
            yield
            eb = ebs[s]
            act(eb, bb[:, 0:128], AF.Exp)
            enb = r_enb()
            act(enb, bb[:, 0:128], AF.Exp, scale=-1.0)
            ec = r_ec()
            act(ec, bb[:, 128:256], AF.Exp)
            yield
            for hh in range(4):
                ps = slice(32 * hh, 32 * hh + 32)
                stt("dve", qblks[s][ps, hh, :], gqT[ps, ssl], 32.0 ** -0.5, eb[ps, :], ALU.mult, ALU.mult)
            kt = r_kt()
            tt("pool", kt, gkT[:, ssl], enb, ALU.mult)
            tt("pool", khs[s], gkt[:, s, :], ec, ALU.mult)
            yield
            ab = bank()
            mm(ab, kt, qblks[s].rearrange("p h i -> p (h i)"), True, True)
            yield
            tt("dve", atms[s].rearrange("p (h i) -> p h i", h=4), ab.rearrange("p (h i) -> p h i", h=4),
               mask_b.unsqueeze(1).broadcast_to([128, 4, 128]), ALU.mult,
               rd=[ab, mask_b], wr=[atms[s]])
            yield

        NA = 9

        def gla_B(s, bank):
            ssl = slice(s * 128, (s + 1) * 128)
            atm = atms[s]
            ob = bank()
            for hh in range(4):
                mm(ob[:, hh * 64:(hh + 1) * 64], atm[:, hh * 128:(hh + 1) * 128], gv[:, s, hh * 64:(hh + 1) * 64],
                   True, False)
                mm(ob[:, hh * 64:(hh + 1) * 64], qblks[s][:, hh, :], Sbf[l][:, hh * 64:(hh + 1) * 64], False, True)
            ub = bank()
            mm(ub[:, 0:256], khs[s], gv[:, s, :], True, True)
            yield
            stt("dve", S32[l], S32[l], ebs[s][:, 127:128], ub[:, 0:256], ALU.mult, ALU.add)
            osb = r_osb()
            cp("act", osb, ob[:, 0:256])
            yield
            cp("pool", Sbf[l], S32[l])
            osq = r_osq()
            tt("pool", osq, osb, osb, ALU.mult)
            yield
            ssq = r_st4()
            P.op("dve", lambda h, ssq=ssq, osq=osq: h.reduce_sum(
                out=ssq, in_=osq.rearrange("p (h d) -> p h d", h=4), axis=AX.X),
                reads=[osq], writes=[ssq])
            yield
            lnv = r_st4()
            act(lnv, ssq, AF.Ln, bias=1e-6, scale=1.0 / 64.0)
            yield
            rn = r_st4()
            act(rn, lnv, AF.Exp, scale=-0.5)
            yield
            tt("pool", osb.rearrange("p (h d) -> p h d", h=4), osb.rearrange("p (h d) -> p h d", h=4),
               rn.unsqueeze(2).broadcast_to([128, 4, 64]), ALU.mult, rd=[osb, rn], wr=[osb])
            yield
            og = r_og()
            tt("pool", og, osb, grs[:, s, :], ALU.mult)
            yield
            tb = bank().bitcast(BF16)
            for c in range(2):
                tr(tb[:, c * 128:(c + 1) * 128], og[:, c * 128:(c + 1) * 128], ident_b)
            yield
            cp("act", mixg[:, :, ssl], tb[:, 0:256].rearrange("p (c t) -> p c t", c=2),
               rd=[tb], wr=[mixg[:, 0, ssl], mixg[:, 1, ssl]])
            yield

        def swa_chain(n, kv, bank):
            qsl = slice(n * 128, (n + 1) * 128)
            pbf = r_pb()
            sbks, ss_, negms, rsums, rdens = [], [], [], [], []
            for pr in range(2):
                sbk = bank()
                for gi in range(2):
                    hh = kv * 4 + pr + 2 * gi
                    po = (hh % 2) * 64
                    mm(sbk[:, gi * 256:(gi + 1) * 256], sqT[po:po + 64, hh // 2, qsl],
                       skT[po:po + 64, kv, n * 128:n * 128 + 256], True, True)
                sbks.append(sbk)
            yield
            for pr in range(2):
                h0 = kv * 4 + pr * 2
                s_ = r_s()
                stt("dve", s_, sbks[pr].rearrange("p (g k) -> p g k", g=2), 0.125, swab[:, h0:h0 + 2, :],
                    ALU.mult, ALU.add, rd=[sbks[pr], swab[:, h0, :], swab[:, h0 + 1, :]], wr=[s_])
                if very_first and n == 0:
                    memset("dve", s_[:, :, 0:128], NEG, wr=[s_])
                ss_.append(s_)
            yield
            for pr in range(2):
                h0 = kv * 4 + pr * 2
                s_ = ss_[pr]
                m = r_st2()
                P.op("dve", lambda h, m=m, s_=s_: h.reduce_max(out=m, in_=s_, axis=AX.X),
                     reads=[s_], writes=[m])
                negm = r_st2()
                stt("dve", negm, m, -1.0, nsink[l][:, h0:h0 + 2], ALU.mult, ALU.min)
                negms.append(negm)
            yield
            esks = []
            for pr in range(2):
                h0 = kv * 4 + pr * 2
                dsk = r_st2()
                tt("dve", dsk, pb[l][:, 256 + h0:256 + h0 + 2], negms[pr], ALU.add)
                esk = r_st2()
                act(esk, dsk, AF.Exp)
                esks.append(esk)
                rsum = r_st2()
                for gi in range(2):
                    act(ss_[pr][:, gi, :], ss_[pr][:, gi, :], AF.Exp, bias=negms[pr][:, gi:gi + 1],
                        accum=rsum[:, gi:gi + 1], rd=[ss_[pr]], wr=[ss_[pr]])
                rsums.append(rsum)
            yield
            for pr in range(2):
                den = r_st2()
                tt("dve", den, rsums[pr], esks[pr], ALU.add)
                rden = r_st2()
                recip(rden, den)
                rdens.append(rden)
            yield
            for pr in range(2):
                for gi in range(2):
                    act(pbf[:, pr + 2 * gi, :], ss_[pr][:, gi, :], AF.Identity, scale=rdens[pr][:, gi:gi + 1],
                        rd=[ss_[pr]], wr=[pbf])
            yield
            tb = bank().bitcast(BF16)
            for kb in range(2):
                for g in range(4):
                    tr(tb[:, kb * 512 + g * 128:kb * 512 + (g + 1) * 128],
                       pbf[:, g, kb * 128:(kb + 1) * 128], ident_b)
            yield
            pT = r_pT()
            cp("act", pT.rearrange("p k q -> p (k q)"), tb, rd=[tb], wr=[pT])
            yield
            ob = bank()
            for kb in range(2):
                mm(ob[0:64, :], sv[:, n + kb, kv * 64:(kv + 1) * 64], pT[:, kb, :], kb == 0, kb == 1)
            yield
            cp("act", mixs[:, kv * 4:(kv + 1) * 4, qsl], ob[0:64, :].rearrange("p (g q) -> p g q", g=4),
               rd=[ob], wr=[mixs[:, kv * 4 + g, qsl] for g in range(4)])
            yield

        def seq(*gs):
            for g in gs:
                yield from g

        def delay(k):
            for _ in range(k):
                yield

        def lane_banks(idxs):
            st_ = {"i": 0}

            def nxt():
                b_ = banks[idxs[st_["i"] % len(idxs)]]
                st_["i"] += 1
                return b_
            return nxt

        bA0, bA1 = lane_banks([0]), lane_banks([1])
        bX, bY = lane_banks([2, 3]), lane_banks([4, 5])
        bC = lane_banks([6, 7])
        lanes = [
            seq(gla_A(0, bA0), gla_A(2, bA0)),
            seq(gla_A(1, bA1), gla_A(3, bA1)),
            seq(*[swa_chain(n_, 0, bX) for n_ in range(4)]),
            seq(*[swa_chain(n_, 1, bY) for n_ in range(4)]),
            conv_ln_lane(bC),
            seq(delay(2 * NA), *[gla_B(s_, bC) for s_ in range(4)]),
        ]
        while lanes:
            nxt = []
            for g in lanes:
                try:
                    next(g)
                    nxt.append(g)
                except StopIteration:
                    pass
            lanes = nxt
        cp("pool", kprev[l], skT[:, :, 512:640])
        cp("pool", vprev[l], sv[:, 4, :])
        for c in range(2):
            act(mixc[:, c, :], ln_t1[c], AF.Silu, bias=ppl[:, PP_LB + c:PP_LB + c + 1],
                scale=ppl[:, PP_LG + c:PP_LG + c + 1])

        for dc in range(KD):
            mun.PF = 2
            slot = mun.get(10 + dc)
            wv = slot[:, 0:1536].rearrange("p (k n) -> p k n", k=12)
            Y = bank()
            for kc in range(12):
                if kc < 2:
                    mm(Y, wv[:, kc, :], mixg[:, kc, :], kc == 0, False)
                elif kc < 4:
                    mm(Y, wv[:, kc, :], mixc[:, kc - 2, :], False, False)
                else:
                    mm(Y, wv[0:64, kc, :], mixs[:, kc - 4, :], False, kc == 11)
            sl = slice(tok0, tok0 + 512)
            cp("dve", hb[:, dc, sl], Y)
            act(xn[:, dc, sl], hb[:, dc, sl], AF.Square)
        postnorm(l, 3, half, False)

    for t in range(NT):
        first = (t == 0)
        for c in range(KD):
            dma(X[:, c, :], xT[c * 128:(c + 1) * 128, t * T:(t + 1) * T], "xin")
        for l in range(L):
            if dbg == ("init", l):
                break
            build_cdiag(l)
            ffn(l, 0, first)
            if dbg == ("ffn1", l):
                break
            prenorm(l, 2)
            for half in range(NTC):
                mixer_half(l, t, half, first)
            if dbg == ("mix", l):
                break
            ffn(l, 1, first)
        if first:
            flush_stores()
        for c in range(KD):
            dma(outT[c * 128:(c + 1) * 128, t * T:(t + 1) * T], X[:, c, :], "xout")

    with nc.allow_low_precision("bf16 matmuls with fp32 accumulation"):
        P.emit(final_wait_chans=["xout"])
    if os.environ.get("K_VERBOSE"):
        print("sbuf bytes remaining", nc.sbuf_bytes_remaining, {e: len(v) for e, v in P.ops.items()})
    return nc, P


_CACHE = {}


def _run(inp, S, L, n_cores, dbg=None, trace=False):
    key = (S, L, dbg)
    if key not in _CACHE:
        _CACHE[key] = build(S, L, dbg=dbg)
    nc, _ = _CACHE[key]
    wts = _prep_weights(inp, L)
    cst = _const_table()
    x = np.asarray(inp["x"], np.float32)
    in_maps = []
    for b in range(n_cores):
        m = dict(wts)
        m["cst"] = cst
        m["xT"] = np.ascontiguousarray(x[b].T)
        in_maps.append(m)
    res = run_bass_kernel_spmd(nc, in_maps, core_ids=list(range(n_cores)), trace=trace)
    out = np.stack([np.ascontiguousarray(r["outT"].T) for r in res.results], 0)
    return out, res


def kernel(x, norm_g, ffn_w_gate, ffn_w_up, ffn_w_down, w_in, gla_w_gate2, gla_b_gate,
           gla_norm_g, conv_w, conv_b, conv_ln_g, conv_ln_b, swa_sinks, w_out):
    inp = dict(x=x, norm_g=norm_g, ffn_w_gate=ffn_w_gate, ffn_w_up=ffn_w_up, ffn_w_down=ffn_w_down,
               w_in=w_in, gla_w_gate2=gla_w_gate2, gla_b_gate=gla_b_gate, gla_norm_g=gla_norm_g,
               conv_w=conv_w, conv_b=conv_b, conv_ln_g=conv_ln_g, conv_ln_b=conv_ln_b,
               swa_sinks=swa_sinks, w_out=w_out)
    inp = {k: np.asarray(v, np.float32) for k, v in inp.items()}
    B, S, _ = inp["x"].shape
    L = inp["norm_g"].shape[0]
    out, _ = _run(inp, S, L, B)
    return out.astype(np.float32)
```
